# Optimizing a Trainium2 kernel written in Bass

```python
import jax
import jax.numpy as jnp
from jax import lax
import numpy as np

D_MODEL = 2048
BATCH = 4
SEQ = 8192
DEPTH = 1

ATTN_HEAD_DIM = 128
ATTN_WIDTH = D_MODEL // 2
ATTN_HEADS = ATTN_WIDTH // ATTN_HEAD_DIM
RET_HEAD_DIM = 256
RET_WIDTH = D_MODEL - ATTN_WIDTH
RET_HEADS = RET_WIDTH // RET_HEAD_DIM
MIX_WIDTH = ATTN_WIDTH + RET_WIDTH
IN_COLS = 3 * ATTN_WIDTH + 4 * RET_WIDTH
DILATED_PATTERNS = ((128, 1), (512, 4), (2048, 16))
ROPE_THETA = 500000.0
ROPE_DIM = ATTN_HEAD_DIM // 4
RET_THETA = 10000.0
RET_CHUNK = 128
N_GROUPS = 4
EXPERTS_PER_GROUP = 8
N_EXPERTS = N_GROUPS * EXPERTS_PER_GROUP
TOP_K_IN_GROUP = 2
EXPERT_FF = D_MODEL // 4
MOE_BLOCK = 128
DEEPNORM_ALPHA = (2 * DEPTH) ** 0.25
DEEPNORM_BETA = (8 * DEPTH) ** -0.25
LN_EPS = 1e-5
MASK_VALUE = -1e30

kernel_name = 'hybrid_dilated_retention_hmoe_encoder'


def layer_norm(x, g=None, b=None):
    xf = x.astype(jnp.float32)
    mu = jnp.mean(xf, -1, keepdims=True)
    var = jnp.mean(jnp.square(xf - mu), -1, keepdims=True)
    y = (xf - mu) * lax.rsqrt(var + LN_EPS)
    if g is not None:
        y = y * g + b
    return y.astype(x.dtype)


def rotary(x, pos, inv_freq):
    n = inv_freq.shape[0]
    ang = pos.astype(jnp.float32)[:, None, :, None] * inv_freq
    cos, sin = jnp.cos(ang), jnp.sin(ang)
    xf = x.astype(jnp.float32)
    x1, x2, rest = xf[..., :n], xf[..., n:2 * n], xf[..., 2 * n:]
    out = jnp.concatenate([x1 * cos - x2 * sin, x2 * cos + x1 * sin, rest], axis=-1)
    return out.astype(x.dtype)


def banded_softmax_attention(q, k, v, half):
    lead = q.shape[:-2]
    L, hd = q.shape[-2], q.shape[-1]
    W = half
    nb = -(-L // W)
    Lp = nb * W
    nl = len(lead)
    qp = jnp.pad(q, [(0, 0)] * nl + [(0, Lp - L), (0, 0)]).reshape(*lead, nb, W, hd)
    kv_pad = [(0, 0)] * nl + [(W, Lp - L + W), (0, 0)]
    kp = jnp.pad(k, kv_pad)
    vp = jnp.pad(v, kv_pad)

    def key_blocks(t):
        return jnp.concatenate([t[..., o * W:(o + nb) * W, :].reshape(*lead, nb, W, hd) for o in range(3)], axis=-2)

    kb, vb = key_blocks(kp), key_blocks(vp)
    qpos = jnp.arange(Lp).reshape(nb, W, 1)
    kpos = (jnp.arange(nb) * W - W)[:, None, None] + jnp.arange(3 * W)[None, None, :]
    mask = (jnp.abs(qpos - kpos) <= half) & (kpos >= 0) & (kpos < L)
    s = jnp.einsum('...nqd,...nkd->...nqk', qp, kb).astype(jnp.float32)
    s = jnp.where(mask, s, MASK_VALUE)
    m = jnp.max(s, -1, keepdims=True)
    p = jnp.exp(s - m)
    den = jnp.sum(p, -1)
    o = jnp.einsum('...nqk,...nkd->...nqd', p, vb.astype(jnp.float32)) / den[..., None]
    lse = m[..., 0] + jnp.log(den)
    o = o.reshape(*lead, Lp, hd)[..., :L, :]
    lse = lse.reshape(*lead, Lp)[..., :L]
    return o, lse


def dilated_mixture_attention(q, k, v):
    B, H, S, hd = q.shape
    outs, lses = [], []
    for window, dil in DILATED_PATTERNS:
        half = window // (2 * dil)

        def to_res(t):
            return t.reshape(B, H, S // dil, dil, hd).transpose(0, 1, 3, 2, 4)

        o, lse = banded_softmax_attention(to_res(q), to_res(k), to_res(v), half)
        outs.append(o.transpose(0, 1, 3, 2, 4).reshape(B, H, S, hd))
        lses.append(lse.transpose(0, 1, 3, 2).reshape(B, H, S))
    w = jax.nn.softmax(jnp.stack(lses), axis=0)
    out = jnp.einsum('pbhs,pbhsd->bhsd', w, jnp.stack(outs))
    return out.astype(q.dtype)


def retention_one_direction(q, k, v, log_decay, inclusive):
    B, H, S, dh = q.shape
    C = RET_CHUNK
    N = S // C
    lg = log_decay.astype(jnp.float32)
    idx = jnp.arange(C, dtype=jnp.float32)
    diff = idx[:, None] - idx[None, :]
    keep = diff >= 0 if inclusive else diff > 0
    dmat = jnp.where(keep, jnp.exp(lg[:, None, None] * jnp.maximum(diff, 0.0)), 0.0)
    qc, kc, vc = [t.astype(jnp.float32).reshape(B, H, N, C, dh) for t in (q, k, v)]
    scores = jnp.einsum('bhnid,bhnjd->bhnij', qc, kc) * dmat[None, :, None]
    y_intra = jnp.einsum('bhnij,bhnjd->bhnid', scores, vc)
    zeta = jnp.exp(lg[:, None] * (C - 1 - idx))
    xi = jnp.exp(lg[:, None] * (idx + 1))
    chunk_decay = jnp.exp(lg * C)

    def step(state, xs):
        qn, kn, vn = xs
        y = jnp.einsum('bhcd,bhde->bhce', qn, state) * xi[None, :, :, None]
        state = state * chunk_decay[None, :, None, None] + jnp.einsum('bhcd,bhce->bhde', kn * zeta[None, :, :, None], vn)
        return state, y

    xs = tuple(jnp.moveaxis(t, 2, 0) for t in (qc, kc, vc))
    _, y_cross = lax.scan(step, jnp.zeros((B, H, dh, dh), jnp.float32), xs)
    return (y_intra + jnp.moveaxis(y_cross, 0, 2)).reshape(B, H, S, dh)


def bidirectional_retention(q, k, v, log_decay_f, log_decay_b):
    y_f = retention_one_direction(q, k, v, log_decay_f, True)
    flip = lambda t: jnp.flip(t, axis=2)
    y_b = flip(retention_one_direction(flip(q), flip(k), flip(v), log_decay_b, False))
    return layer_norm(y_f + y_b)


def hierarchical_moe(h, w_group, b_group, w_sub, b_sub, w1, w3, w2):
    B, S, D = h.shape
    T = B * S
    A = T * TOP_K_IN_GROUP
    ht = h.reshape(T, D)
    glog = (ht @ w_group + b_group).astype(jnp.float32)
    gprob = jax.nn.softmax(glog, axis=-1)
    gsel = jnp.argmax(glog, axis=-1)
    pg = jnp.take_along_axis(gprob, gsel[:, None], axis=1)[:, 0]
    slog_all = jnp.einsum('td,gde->tge', ht, w_sub) + b_sub
    slog = jnp.take_along_axis(slog_all, gsel[:, None, None], axis=1)[:, 0].astype(jnp.float32)
    top_v, top_i = lax.top_k(slog, TOP_K_IN_GROUP)
    top_w = jax.nn.softmax(top_v, axis=-1) * pg[:, None]
    eid = (gsel[:, None] * EXPERTS_PER_GROUP + top_i).reshape(-1).astype(jnp.int32)
    wt = top_w.reshape(-1)
    tok = jnp.repeat(jnp.arange(T, dtype=jnp.int32), TOP_K_IN_GROUP)
    order = jnp.argsort(eid)
    e_sorted = eid[order]
    counts = jnp.bincount(eid, length=N_EXPERTS)
    padded = (counts + MOE_BLOCK - 1) // MOE_BLOCK * MOE_BLOCK
    pend = jnp.cumsum(padded)
    pstart = pend - padded
    start = jnp.cumsum(counts) - counts
    rank = jnp.arange(A) - start[e_sorted]
    dest = pstart[e_sorted] + rank
    P = A + N_EXPERTS * MOE_BLOCK
    nblk = P // MOE_BLOCK
    buf_tok = jnp.zeros((P,), jnp.int32).at[dest].set(tok[order])
    buf_w = jnp.zeros((P,), jnp.float32).at[dest].set(wt[order])
    blk_e = jnp.minimum(jnp.searchsorted(pend, jnp.arange(nblk) * MOE_BLOCK, side='right'), N_EXPERTS - 1)
    xb = ht[buf_tok].reshape(nblk, MOE_BLOCK, D)

    def expert_block(args):
        xblk, e = args
        return (jax.nn.silu(xblk @ w1[e]) * (xblk @ w3[e])) @ w2[e]

    yb = lax.map(expert_block, (xb, blk_e)).reshape(P, D)
    out = jnp.zeros((T, D), jnp.float32).at[buf_tok].add(yb.astype(jnp.float32) * buf_w[:, None])
    return out.reshape(B, S, D).astype(h.dtype)


def setup_inputs(seed: int = 0) -> dict:
    key = jax.random.key(seed)
    ks = jax.random.split(key, 24)
    f32 = jnp.float32
    D = D_MODEL

    def nrm(k, shape, scale):
        return jax.random.normal(k, shape, f32) * scale

    x = nrm(ks[0], (BATCH, SEQ, D), 1.0)
    c = nrm(ks[1], (BATCH, D), 1.0)
    positions = (jnp.cumsum(jax.random.randint(ks[2], (BATCH, SEQ), 1, 3), axis=1) - 1).astype(jnp.int32)
    ln0_g = 1.0 + nrm(ks[3], (D,), 0.02)
    ln0_b = nrm(ks[4], (D,), 0.02)
    w_ada = nrm(ks[5], (DEPTH, D, 6 * D), 0.2 * D ** -0.5)
    b_ada = nrm(ks[6], (DEPTH, 6 * D), 0.01)
    col_scale = jnp.concatenate([
        jnp.ones((2 * ATTN_WIDTH,), f32), jnp.full((ATTN_WIDTH,), DEEPNORM_BETA, f32),
        jnp.ones((2 * RET_WIDTH,), f32), jnp.full((RET_WIDTH,), DEEPNORM_BETA, f32),
        jnp.ones((RET_WIDTH,), f32)])
    w_in = nrm(ks[7], (DEPTH, D, IN_COLS), D ** -0.5) * col_scale
    w_out = nrm(ks[8], (DEPTH, MIX_WIDTH, D), DEEPNORM_BETA * MIX_WIDTH ** -0.5)
    base = jnp.log(1.0 - 2.0 ** (-5.0 - jnp.arange(RET_HEADS, dtype=f32)))
    ret_log_decay_f = base * jnp.exp(nrm(ks[9], (DEPTH, RET_HEADS), 0.1))
    ret_log_decay_b = base * jnp.exp(nrm(ks[10], (DEPTH, RET_HEADS), 0.1))
    ln1_g = 1.0 + nrm(ks[11], (DEPTH, D), 0.02)
    ln1_b = nrm(ks[12], (DEPTH, D), 0.02)
    w_group = nrm(ks[13], (DEPTH, D, N_GROUPS), D ** -0.5)
    b_group = nrm(ks[14], (DEPTH, N_GROUPS), 0.01)
    w_sub = nrm(ks[15], (DEPTH, N_GROUPS, D, EXPERTS_PER_GROUP), D ** -0.5)
    b_sub = nrm(ks[16], (DEPTH, N_GROUPS, EXPERTS_PER_GROUP), 0.01)
    w1 = nrm(ks[17], (DEPTH, N_EXPERTS, D, EXPERT_FF), D ** -0.5)
    w3 = nrm(ks[18], (DEPTH, N_EXPERTS, D, EXPERT_FF), DEEPNORM_BETA * D ** -0.5)
    w2 = nrm(ks[19], (DEPTH, N_EXPERTS, EXPERT_FF, D), DEEPNORM_BETA * EXPERT_FF ** -0.5)
    ln2_g = 1.0 + nrm(ks[20], (DEPTH, D), 0.02)
    ln2_b = nrm(ks[21], (DEPTH, D), 0.02)
    return {'x': x, 'c': c, 'positions': positions, 'ln0_g': ln0_g, 'ln0_b': ln0_b,
            'w_ada': w_ada, 'b_ada': b_ada, 'w_in': w_in, 'w_out': w_out,
            'ret_log_decay_f': ret_log_decay_f, 'ret_log_decay_b': ret_log_decay_b,
            'ln1_g': ln1_g, 'ln1_b': ln1_b, 'w_group': w_group, 'b_group': b_group,
            'w_sub': w_sub, 'b_sub': b_sub, 'w1': w1, 'w3': w3, 'w2': w2,
            'ln2_g': ln2_g, 'ln2_b': ln2_b}


def reference(x, c, positions, ln0_g, ln0_b, w_ada, b_ada, w_in, w_out, ret_log_decay_f, ret_log_decay_b,
              ln1_g, ln1_b, w_group, b_group, w_sub, b_sub, w1, w3, w2, ln2_g, ln2_b):
    B, S, _ = x.shape
    x = layer_norm(x, ln0_g, ln0_b)
    inv_rope = ROPE_THETA ** (-jnp.arange(0, ROPE_DIM, 2, dtype=jnp.float32) / ROPE_DIM)
    inv_ret = RET_THETA ** (-jnp.linspace(0.0, 1.0, RET_HEAD_DIM // 2, dtype=jnp.float32))
    cs = jax.nn.silu(c)
    A, R = ATTN_WIDTH, RET_WIDTH

    def heads(t, n, d):
        return t.reshape(B, S, n, d).transpose(0, 2, 1, 3)

    for l in range(DEPTH):
        mod = cs @ w_ada[l] + b_ada[l]
        sh1, sc1, g1, sh2, sc2, g2 = [m[:, None, :] for m in jnp.split(mod, 6, axis=-1)]
        h = x * (1.0 + sc1) + sh1
        z = h @ w_in[l]
        aq, ak, av, rq, rk, rv, rg = jnp.split(z, [A, 2 * A, 3 * A, 3 * A + R, 3 * A + 2 * R, 3 * A + 3 * R], axis=-1)
        aq = rotary(heads(aq, ATTN_HEADS, ATTN_HEAD_DIM), positions, inv_rope) * ATTN_HEAD_DIM ** -0.5
        ak = rotary(heads(ak, ATTN_HEADS, ATTN_HEAD_DIM), positions, inv_rope)
        attn = dilated_mixture_attention(aq, ak, heads(av, ATTN_HEADS, ATTN_HEAD_DIM))
        attn = attn.transpose(0, 2, 1, 3).reshape(B, S, A)
        rq = rotary(heads(rq, RET_HEADS, RET_HEAD_DIM), positions, inv_ret)
        rk = rotary(heads(rk, RET_HEADS, RET_HEAD_DIM), positions, inv_ret) * RET_HEAD_DIM ** -0.5
        ret = bidirectional_retention(rq, rk, heads(rv, RET_HEADS, RET_HEAD_DIM), ret_log_decay_f[l], ret_log_decay_b[l])
        ret = jax.nn.silu(rg) * ret.transpose(0, 2, 1, 3).reshape(B, S, R).astype(rg.dtype)
        mix = jnp.concatenate([attn, ret], axis=-1) @ w_out[l]
        x = layer_norm(DEEPNORM_ALPHA * x + (1.0 + g1) * mix, ln1_g[l], ln1_b[l])
        h = x * (1.0 + sc2) + sh2
        ffn = hierarchical_moe(h, w_group[l], b_group[l], w_sub[l], b_sub[l], w1[l], w3[l], w2[l])
        x = layer_norm(DEEPNORM_ALPHA * x + (1.0 + g2) * ffn, ln2_g[l], ln2_b[l])
    return x
```

```python
import contextlib
import os
KSKIP = set(os.environ.get('KSKIP', '').split(','))
KTILES = int(os.environ.get('KTILES', '0'))
import math
import numpy as np
import ml_dtypes
import concourse.bass as bass
import concourse.mybir as mybir
from concourse.bass_utils import run_bass_kernel_spmd

F32 = mybir.dt.float32
BF16 = mybir.dt.bfloat16
I32 = mybir.dt.int32
AF = mybir.ActivationFunctionType
OP = mybir.AluOpType
AX = mybir.AxisListType

D = 2048
KC = 16
NHALO = 8
LN_EPS = 1e-5
ALPHA = 2.0 ** 0.25
NEG = -30000.0


class Cfg:
    def __init__(self, NT=32, FF=512, CAPB=4, B=4):
        self.NT = NT
        self.NO = NT
        self.FF = FF
        self.CAPB = CAPB
        self.B = B
        self.SL = NT * 128
        self.SA = self.SL + 2048
        self.NE = 32
        self.CAP = CAPB * 128


class Tick:
    __slots__ = ("key", "sem", "val")

    def __init__(self, key, sem, val):
        self.key = key
        self.sem = sem
        self.val = val


class Buf:
    def __init__(self, h, name=""):
        self.h = h
        self.name = name
        self.w = None
        self.r = {}
        self.psum = False

    def __getitem__(self, idx):
        return self.h[idx]


class Eng:
    def __init__(self, key, eng, sem, is_pe=False):
        self.key = key
        self.eng = eng
        self.sem = sem
        self.cnt = 0
        self.seen = {}
        self.is_pe = is_pe
        self.dsems = []
        self.dvals = []
        self.dma_i = 0


NDS = 20


class K:
    def __init__(self, nc):
        self.nc = nc
        self.es = contextlib.ExitStack()
        self.E = {}
        for key, eng, pe in (("pe", nc.tensor, True), ("act", nc.scalar, False), ("dve", nc.vector, False),
                             ("pool", nc.gpsimd, False), ("sp", nc.sync, False)):
            sem = self.es.enter_context(nc.semaphore("s_" + key))
            self.E[key] = Eng(key, eng, sem, pe)
        for q in ("sp", "act", "pool"):
            e = self.E[q]
            for i in range(NDS):
                e.dsems.append(self.es.enter_context(nc.semaphore("d_%s%d" % (q, i))))
                e.dvals.append(0)
        self.scopes = []
        self.uid = 0

    def _stack(self):
        return self.scopes[-1] if self.scopes else self.es

    @contextlib.contextmanager
    def scope(self):
        st = contextlib.ExitStack()
        self.scopes.append(st)
        try:
            yield
            self.fence()
        finally:
            self.scopes.pop()
            st.close()

    def sb(self, name, shape, dt):
        self.uid += 1
        h = self._stack().enter_context(self.nc.sbuf_tensor("%s_%d" % (name, self.uid), list(shape), dt))
        return Buf(h, name)

    def ps(self, name, shape, dt):
        self.uid += 1
        h = self._stack().enter_context(self.nc.psum_tensor("%s_%d" % (name, self.uid), list(shape), dt))
        b = Buf(h, name)
        b.psum = True
        return b

    def _wait(self, E, ticks):
        need = {}
        for t in ticks:
            if t is None:
                continue
            if E.is_pe and t.key == E.key:
                continue
            cur = need.get(t.key)
            if cur is None or cur[1] < t.val:
                need[t.key] = (t.sem, t.val)
        for key, (sem, val) in need.items():
            if E.seen.get(key, 0) >= val:
                continue
            E.eng.wait_ge(sem, val)
            E.seen[key] = val

    def _deps(self, E, R, W):
        deps = []
        for b in R:
            deps.append(b.w)
            if b.psum:
                for k, t in b.r.items():
                    if k != E.key:
                        deps.append(t)
        for b in W:
            deps.append(b.w)
            for k, t in b.r.items():
                deps.append(t)
        return deps

    def _commit(self, E, t, R, W):
        for b in R:
            if b not in W:
                b.r[E.key if not isinstance(t.key, tuple) else t.key] = t
        for b in W:
            b.w = t
            b.r = {}

    def op(self, en, fn, R=(), W=()):
        E = self.E[en]
        self._wait(E, self._deps(E, R, W))
        ins = fn(E.eng)
        E.cnt += 1
        ins.then_inc(E.sem, 1)
        t = Tick(E.key, E.sem, E.cnt)
        self._commit(E, t, R, W)
        return t

    def dma(self, q, out, in_, R=(), W=(), indirect=None, **kw):
        E = self.E[q]
        self._wait(E, self._deps(E, R, W))
        slot = E.dma_i % NDS
        E.dma_i += 1
        sem = E.dsems[slot]
        prev = E.dvals[slot]
        key = ("d", q, slot)
        if prev > 0 and E.seen.get(key, 0) < prev:
            E.eng.wait_ge(sem, prev)
            E.seen[key] = prev
        if indirect is None:
            ins = E.eng.dma_start(out=out, in_=in_, **kw)
        else:
            ins = E.eng.indirect_dma_start(out=out, in_=in_, **indirect)
        ins.then_inc(sem, 16)
        E.dvals[slot] = prev + 16
        t = Tick(key, sem, prev + 16)
        self._commit(E, t, R, W)
        return t

    def fence(self):
        ticks = []
        for key, e in self.E.items():
            if e.cnt > 0:
                ticks.append(Tick(e.key, e.sem, e.cnt))
            for i, v in enumerate(e.dvals):
                if v > 0:
                    ticks.append(Tick(("d", key, i), e.dsems[i], v))
        for key, e in self.E.items():
            self._wait(e, [t for t in ticks if not (t.key == e.key)])

    def close(self):
        self.fence()
        self.es.close()


def _bf(a):
    return np.asarray(a, dtype=np.float32).astype(ml_dtypes.bfloat16)


def host_consts(cfg, hf):
    c = {}
    a = np.arange(128)[:, None]
    i = np.arange(128)[None, :]
    mA = (a >= i)
    mB = (a <= i)
    maskL = 1.0 if hf == 1 else 0.0
    maskR = 1.0 if hf == 0 else 0.0
    mAe = mA & ((a >= 64) | (maskL > 0))
    mBe = mB & ((a < 64) | (maskR > 0))
    neg = np.stack([np.where(m, 0.0, NEG) for m in (mA, mB, mAe, mBe)], axis=1)
    c["negmask"] = _bf(neg)
    c["ident_f"] = np.eye(128, dtype=np.float32)
    c["ident_b"] = _bf(np.eye(128))
    inv_rope = (500000.0 ** (-np.arange(0, 32, 2, dtype=np.float32) / 32.0)).astype(np.float32)
    inv_ret = (10000.0 ** (-np.linspace(0.0, 1.0, 128, dtype=np.float32))).astype(np.float32)
    invf = np.concatenate([inv_ret, inv_rope]).astype(np.float32) / np.float32(2.0 * math.pi)
    c["invf"] = np.broadcast_to(invf[None, :], (128, 144)).astype(np.float32).copy()
    idx = np.arange(128, dtype=np.float32)
    dm = idx[None, :] - idx[:, None]
    c["dpos"] = np.maximum(dm, 0.0).astype(np.float32)
    c["dneg"] = np.maximum(-dm, 0.0).astype(np.float32)
    c["mfw"] = (dm >= 0).astype(np.float32)
    c["mbw"] = (dm < 0).astype(np.float32)
    c["idxcol"] = idx[:, None].astype(np.float32).copy()
    c["idxrow"] = np.broadcast_to(idx[None, :], (128, 128)).astype(np.float32).copy()
    c["flags"] = np.array([[1.0 if hf == 1 else 0.0, 1.0 if hf == 0 else 0.0]], np.float32).repeat(128, 0)
    c["tri"] = (np.arange(128)[:, None] < np.arange(128)[None, :]).astype(np.float32)
    c["ones_f"] = np.ones((128, 128), np.float32)
    c["iota_e"] = np.broadcast_to(np.arange(32, dtype=np.float32)[None, :], (128, 32)).copy()
    c["tokid"] = (np.arange(cfg.NT)[None, :] * 128 + np.arange(128)[:, None]).astype(np.int32)
    return c


def prep_inputs(inp, cfg):
    x = np.asarray(inp["x"])
    B, S, _ = x.shape
    SL = cfg.SL
    assert S == 2 * SL
    pos = np.asarray(inp["positions"])
    maps = []
    f32 = lambda a: np.ascontiguousarray(np.asarray(a, dtype=np.float32))
    shared = {
        "w_ada": f32(inp["w_ada"][0]), "b_ada": f32(inp["b_ada"][0])[None, :],
        "w_in": f32(inp["w_in"][0]), "w_out": f32(inp["w_out"][0]),
        "ln0_g": f32(inp["ln0_g"])[None, :], "ln0_b": f32(inp["ln0_b"])[None, :],
        "ln1_g": f32(inp["ln1_g"][0])[None, :], "ln1_b": f32(inp["ln1_b"][0])[None, :],
        "ln2_g": f32(inp["ln2_g"][0])[None, :], "ln2_b": f32(inp["ln2_b"][0])[None, :],
        "w1": f32(inp["w1"][0]), "w3": f32(inp["w3"][0]), "w2": f32(inp["w2"][0]),
        "lg_f": f32(inp["ret_log_decay_f"][0])[None, :], "lg_b": f32(inp["ret_log_decay_b"][0])[None, :],
    }
    wr = np.concatenate([f32(inp["w_group"][0]),
                         np.transpose(f32(inp["w_sub"][0]), (1, 0, 2)).reshape(D, 32)], axis=1)
    br = np.concatenate([f32(inp["b_group"][0]), f32(inp["b_sub"][0]).reshape(32)])[None, :]
    shared["w_r"] = np.ascontiguousarray(wr)
    shared["b_r"] = np.ascontiguousarray(br)
    for b in range(B):
        for hf in range(2):
            T0 = hf * SL
            m = dict(shared)
            m["x_own"] = np.ascontiguousarray(x[b, T0:T0 + SL])
            if hf == 0:
                oth = np.arange(SL, 2 * SL)
                dist = (oth - SL).astype(np.float32)
                m["lg_oth"] = shared["lg_b"]
            else:
                oth = np.concatenate([np.arange(SL - 1024, SL), np.arange(0, SL - 1024)])
                dist = (SL - 1 - oth).astype(np.float32)
                m["lg_oth"] = shared["lg_f"]
            m["x_oth"] = np.ascontiguousarray(x[b, oth])
            m["pos_own"] = np.ascontiguousarray(pos[b, T0:T0 + SL].reshape(cfg.NT, 128).T.astype(np.int32))
            m["pos_oth"] = np.ascontiguousarray(pos[b, oth].reshape(cfg.NO, 128).T.astype(np.int32))
            m["dist_oth"] = np.ascontiguousarray(dist.reshape(cfg.NO, 128).T)
            m["c_col"] = np.ascontiguousarray(np.asarray(inp["c"], np.float32)[b].reshape(KC, 128).T)
            m.update(host_consts(cfg, hf))
            maps.append(m)
    return maps


def build(cfg, dbg=None, stop_after=None):
    dbg = dbg or []
    nc = bass.Bass("TRN2", target_bir_lowering=False)
    NT, NO, SL, SA, FF, NE, CAP = cfg.NT, cfg.NO, cfg.SL, cfg.SA, cfg.FF, cfg.NE, cfg.CAP
    FC = FF // 128

    def din(name, shape, dt=F32):
        return nc.dram_tensor(name, list(shape), dt, kind="ExternalInput").ap()

    def dscr(name, shape, dt):
        kind = "ExternalOutput" if name in dbg else "Internal"
        return nc.dram_tensor(name, list(shape), dt, kind=kind).ap()

    I = {}
    I["x_own"] = din("x_own", [SL, D])
    I["x_oth"] = din("x_oth", [NO * 128, D])
    I["pos_own"] = din("pos_own", [128, NT], I32)
    I["pos_oth"] = din("pos_oth", [128, NO], I32)
    I["dist_oth"] = din("dist_oth", [128, NO])
    I["lg_oth"] = din("lg_oth", [1, 4])
    I["lg_f"] = din("lg_f", [1, 4])
    I["lg_b"] = din("lg_b", [1, 4])
    I["c_col"] = din("c_col", [128, KC])
    I["w_ada"] = din("w_ada", [D, 6 * D])
    I["b_ada"] = din("b_ada", [1, 6 * D])
    I["w_in"] = din("w_in", [D, 7168])
    I["w_out"] = din("w_out", [D, D])
    for n in ("ln0_g", "ln0_b", "ln1_g", "ln1_b", "ln2_g", "ln2_b"):
        I[n] = din(n, [1, D])
    I["w1"] = din("w1", [NE, D, FF])
    I["w3"] = din("w3", [NE, D, FF])
    I["w2"] = din("w2", [NE, FF, D])
    I["w_r"] = din("w_r", [D, 36])
    I["b_r"] = din("b_r", [1, 36])
    I["negmask"] = din("negmask", [128, 4, 128], BF16)
    I["ident_f"] = din("ident_f", [128, 128])
    I["ident_b"] = din("ident_b", [128, 128], BF16)
    I["invf"] = din("invf", [128, 144])
    for n in ("dpos", "dneg", "mfw", "mbw", "idxrow", "tri", "ones_f"):
        I[n] = din(n, [128, 128])
    I["idxcol"] = din("idxcol", [128, 1])
    I["flags"] = din("flags", [128, 2])
    I["iota_e"] = din("iota_e", [128, 32])
    I["tokid"] = din("tokid", [128, NT], I32)

    out_d = nc.dram_tensor("out", [SL, D], F32, kind="ExternalOutput").ap()

    S = {}
    S["vec"] = dscr("vec", [16, D], F32)
    S["ak"] = dscr("ak", [SA, 1024], BF16)
    S["av"] = dscr("av", [SA, 1024], BF16)
    S["rk"] = dscr("rk", [SL, 1024], BF16)
    S["rv"] = dscr("rv", [SL, 1024], BF16)
    S["aq"] = dscr("aq", [SL, 1024], BF16)
    S["rq"] = dscr("rq", [SL, 1024], BF16)
    S["rg"] = dscr("rg", [SL, 1024], BF16)
    S["xn"] = dscr("xn", [SL, D], F32)
    S["sin"] = dscr("sin", [4, 2, 128, 256], F32)

    k = K(nc)
    op, dma = k.op, k.dma

    ident_f = k.sb("ident_f", [128, 128], F32)
    ident_b = k.sb("ident_b", [128, 128], BF16)
    dma("sp", ident_f[:], I["ident_f"], W=[ident_f])
    dma("sp", ident_b[:], I["ident_b"], W=[ident_b])
    cols = k.sb("cols", [128, 4, KC], F32)

    VEC = {n: i for i, n in enumerate(["ln0_g", "ln0_b", "opg1", "G2", "B2", "ln1_g", "ln1_b", "opg2",
                                       "ln2_g", "ln2_b"])}

    def bc_load(q, dst, src_row):
        return dma(q, dst[:], src_row.partition_broadcast(128), W=[dst])

    NSLOT = NE * CAP
    S["slot"] = dscr("slot", [NSLOT + 128, 128], I32)
    zi = k.sb("zi", [128, 128], I32)
    op("pool", lambda e: e.memset(zi[:], 0), W=[zi])

    with k.scope():
        cc = k.sb("cc", [128, KC], F32)
        dma("sp", cc[:], I["c_col"], W=[cc])
        cs = k.sb("cs", [128, KC], F32)
        op("act", lambda e: e.activation(out=cs[:], in_=cc[:], func=AF.Silu), R=[cc], W=[cs])
        for b in range(NSLOT // 128):
            dma("act", S["slot"][b * 128:(b + 1) * 128, :], zi[:], R=[zi])
        csb = k.sb("csb", [128, KC, 128], BF16)
        op("dve", lambda e: e.tensor_copy(out=csb[:], in_=cs[:].unsqueeze(2).to_broadcast([128, KC, 128])),
           R=[cs], W=[csb])
        mod = k.sb("mod", [128, 6 * D], F32)
        bsl = [k.sb("bsl%d" % i, [128, 512], F32) for i in range(2)]
        wa = [k.sb("wa%d" % i, [128, KC, 512], BF16) for i in range(2)]
        pm = [k.ps("pm%d" % i, [128, 512], F32) for i in range(2)]
        wada_v = I["w_ada"].rearrange("(kc p) n -> p kc n", p=128)
        for s in range(24):
            w = wa[s % 2]
            bs = bsl[s % 2]
            dma("pool", w[:], wada_v[:, :, s * 512:(s + 1) * 512], W=[w])
            dma("sp", bs[:], I["b_ada"][0:1, s * 512:(s + 1) * 512].partition_broadcast(128), W=[bs])
            p = pm[s % 2]
            for kc in range(KC):
                op("pe", lambda e, kc=kc, p=p, w=w: e.matmul(p[:], lhsT=csb[:, kc, :], rhs=w[:, kc, :],
                                                             start=(kc == 0), stop=(kc == KC - 1)),
                   R=[csb, w], W=[p])
            op("dve", lambda e, p=p, s=s, bs=bs: e.tensor_tensor(out=mod[:, s * 512:(s + 1) * 512], in0=p[:], in1=bs[:],
                                                                 op=OP.add), R=[p, bs], W=[mod])
        sh1, sc1, g1, sh2, sc2, g2 = [mod[:, i * D:(i + 1) * D] for i in range(6)]
        lnv = {}
        for n in ("ln0_g", "ln0_b", "ln1_g", "ln1_b"):
            lnv[n] = k.sb(n, [128, D], F32)
            bc_load("sp", lnv[n], I[n])
        t1 = k.sb("t1", [128, D], F32)
        t2 = k.sb("t2", [128, D], F32)
        t3 = k.sb("t3", [128, D], F32)

        def diag_extract(src_ap, srcbufs, ci):
            op("dve", lambda e: e.tensor_tensor(out=t3[:].rearrange("p (c q) -> p c q", q=128),
                                                in0=src_ap.rearrange("p (c q) -> p c q", q=128),
                                                in1=ident_f[:].unsqueeze(1).to_broadcast([128, KC, 128]),
                                                op=OP.mult), R=srcbufs + [ident_f], W=[t3])
            op("dve", lambda e: e.tensor_reduce(out=cols[:, ci, :], in_=t3[:].rearrange("p (c q) -> p c q", q=128),
                                                axis=AX.X, op=OP.add), R=[t3], W=[cols])

        def store_vec(name, src_ap, bufs):
            dma("sp", S["vec"][VEC[name]:VEC[name] + 1, :], src_ap[0:1, :], R=bufs)

        op("dve", lambda e: e.tensor_scalar(out=t1[:], in0=sc1, scalar1=1.0, scalar2=None, op0=OP.add), R=[mod], W=[t1])
        op("dve", lambda e: e.tensor_tensor(out=t2[:], in0=t1[:], in1=lnv["ln0_g"][:], op=OP.mult), R=[t1, lnv["ln0_g"]], W=[t2])
        diag_extract(t2[:], [t2], 0)
        op("dve", lambda e: e.tensor_tensor(out=t2[:], in0=t1[:], in1=lnv["ln0_b"][:], op=OP.mult), R=[t1, lnv["ln0_b"]], W=[t2])
        op("dve", lambda e: e.tensor_tensor(out=t2[:], in0=t2[:], in1=sh1, op=OP.add), R=[t2, mod], W=[t2])
        diag_extract(t2[:], [t2], 1)
        op("dve", lambda e: e.tensor_scalar(out=t1[:], in0=sc2, scalar1=1.0, scalar2=None, op0=OP.add), R=[mod], W=[t1])
        op("dve", lambda e: e.tensor_tensor(out=t2[:], in0=t1[:], in1=lnv["ln1_g"][:], op=OP.mult), R=[t1, lnv["ln1_g"]], W=[t2])
        diag_extract(t2[:], [t2], 2)
        store_vec("G2", t2, [t2])
        t4 = k.sb("t4", [128, D], F32)
        op("dve", lambda e: e.tensor_tensor(out=t4[:], in0=t1[:], in1=lnv["ln1_b"][:], op=OP.mult), R=[t1, lnv["ln1_b"]], W=[t4])
        op("dve", lambda e: e.tensor_tensor(out=t4[:], in0=t4[:], in1=sh2, op=OP.add), R=[t4, mod], W=[t4])
        diag_extract(t4[:], [t4], 3)
        store_vec("B2", t4, [t4])
        t5 = t2
        op("dve", lambda e: e.tensor_scalar(out=t5[:], in0=g1, scalar1=1.0, scalar2=None, op0=OP.add), R=[mod], W=[t5])
        store_vec("opg1", t5, [t5])
        t6 = t4
        op("dve", lambda e: e.tensor_scalar(out=t6[:], in0=g2, scalar1=1.0, scalar2=None, op0=OP.add), R=[mod], W=[t6])
        store_vec("opg2", t6, [t6])
        for n in ("ln0_g", "ln0_b", "ln1_g", "ln1_b", "ln2_g", "ln2_b"):
            dma("sp", S["vec"][VEC[n]:VEC[n] + 1, :], I[n])
    if stop_after == "pre":
        k.close()
        return nc

    win_v = I["w_in"].rearrange("(kc p) n -> p kc n", p=128)

    def load_w(dst, col0, dcol0, ncols):
        for j in range(ncols // 512):
            dma("pool", dst[:, :, dcol0 + j * 512: dcol0 + (j + 1) * 512],
                win_v[:, :, col0 + j * 512: col0 + (j + 1) * 512], W=[dst])

    def phaseA(pass_id):
        with k.scope():
            if pass_id == 1:
                NCOL = 4096
                W = k.sb("WA", [128, KC, NCOL], BF16)
                load_w(W, 1024, 0, 2048)
                load_w(W, 4096, 2048, 2048)
                tiles = [("oth", j) for j in range(NO)] + [("own", i) for i in range(NT)]
            else:
                NCOL = 3072
                W = k.sb("WB", [128, KC, NCOL], BF16)
                load_w(W, 0, 0, 1024)
                load_w(W, 3072, 1024, 1024)
                load_w(W, 6144, 2048, 1024)
                tiles = [("own", i) for i in range(NT)]
                g0 = k.sb("g0", [128, D], F32)
                b0 = k.sb("b0", [128, D], F32)
                bc_load("sp", g0, S["vec"][VEC["ln0_g"]:VEC["ln0_g"] + 1, :])
                bc_load("sp", b0, S["vec"][VEC["ln0_b"]:VEC["ln0_b"] + 1, :])
            if KTILES:
                tiles = [t for t in tiles if t[1] < KTILES or (t[0] == 'oth' and t[1] == NO - 1)]
            invf = k.sb("invf", [128, 144], F32)
            dma("sp", invf[:], I["invf"], W=[invf])
            posi = {"own": k.sb("posi_o", [128, NT], I32), "oth": k.sb("posi_t", [128, NO], I32)}
            posf = {"own": k.sb("posf_o", [128, NT], F32), "oth": k.sb("posf_t", [128, NO], F32)}
            dma("sp", posi["own"][:], I["pos_own"], W=[posi["own"]])
            dma("sp", posi["oth"][:], I["pos_oth"], W=[posi["oth"]])
            for n in ("own", "oth"):
                op("dve", lambda e, n=n: e.tensor_copy(out=posf[n][:], in_=posi[n][:]), R=[posi[n]], W=[posf[n]])
            xt = [k.sb("xt%d" % i, [128, D], F32) for i in range(3)]
            xh = [k.sb("xh%d" % i, [128, D], F32) for i in range(1)]
            hT = [k.sb("hT%d" % i, [128, KC, 128], BF16) for i in range(2)]
            st = k.sb("st", [128, 4, 6], F32)
            mv = k.sb("mv", [128, 2], F32)
            rstd = k.sb("rstd", [128, 1], F32)
            nmr = k.sb("nmr", [128, 1], F32)
            tu = k.sb("tu", [128, 144], F32)
            tk = k.sb("tk", [128, 144], I32)
            tf = k.sb("tf", [128, 144], F32)
            tg = k.sb("tg", [128, 144], F32)
            tabs = [k.sb("tabs%d" % i, [128, 2, 144], F32) for i in range(2)]
            pT = [k.ps("pT%d" % i, [128, 512], F32) for i in range(2)]
            NPZ = 2 if pass_id == 1 else 4
            pz = [k.ps("pz%d" % i, [128, 512], F32) for i in range(NPZ)]
            zo = [k.sb("zo%d" % i, [128, NCOL], BF16) for i in range(2)]
            rt = [k.sb("rt%d" % i, [128, 256], F32) for i in range(4)]
            if pass_id == 1:
                pS = [k.ps("pS%d" % i, [128, 512], F32) for i in range(4)]
                zer = k.sb("zer", [128, 128], BF16)
                op("dve", lambda e: e.memset(zer[:], 0.0), W=[zer])
                for i in range(4):
                    op("pe", lambda e, i=i: e.matmul(pS[i][:], lhsT=zer[:], rhs=W[:, 0, 0:512], start=True, stop=False, skip_group_check=True),
                       R=[zer, W], W=[pS[i]])
                wo = k.sb("wo", [128, NO, 4], F32)
                lgo = k.sb("lgo", [128, 4], F32)
                dist = k.sb("dist", [128, NO], F32)
                bc_load("sp", lgo, I["lg_oth"])
                dma("sp", dist[:], I["dist_oth"], W=[dist])
                for h in range(4):
                    op("dve", lambda e, h=h: e.tensor_scalar(out=wo[:, :, h], in0=dist[:], scalar1=lgo[:, h:h + 1],
                                                             scalar2=None, op0=OP.mult), R=[dist, lgo], W=[wo])
                op("act", lambda e: e.activation(out=wo[:], in_=wo[:], func=AF.Exp), R=[wo], W=[wo])
                kw = [k.sb("kw%d" % i, [128, 1024], BF16) for i in range(2)]
            zc = {"n": 0}

            def stage(ti, part, hook=None):
                kind, idx = tiles[ti]
                src = I["x_own"] if kind == "own" else I["x_oth"]
                X = xt[ti % 3]
                XH = xh[0]
                HT = hT[ti % 2]
                TB = tabs[ti % 2]
                ZO = zo[ti % 2]
                if part == 0:
                    dma("sp", X[:], src[idx * 128:(idx + 1) * 128, :], W=[X])
                elif part == 1:
                    for c4 in range(4):
                        op("dve", lambda e, c4=c4: e.bn_stats(out=st[:, c4, :], in_=X[:, c4 * 512:(c4 + 1) * 512]), R=[X], W=[st])
                    op("dve", lambda e: e.bn_aggr(out=mv[:], in_=st[:].rearrange("p a b -> p (a b)")), R=[st], W=[mv])
                    op("act", lambda e: e.activation(out=rstd[:], in_=mv[:, 1:2], func=AF.Sqrt, bias=LN_EPS_AP[:], scale=1.0),
                       R=[mv, LN_EPS_B], W=[rstd])
                    op("dve", lambda e: e.reciprocal(out=rstd[:], in_=rstd[:]), R=[rstd], W=[rstd])
                    op("dve", lambda e: e.tensor_scalar(out=nmr[:], in0=mv[:, 0:1], scalar1=rstd[:], scalar2=-1.0,
                                                        op0=OP.mult, op1=OP.mult), R=[mv, rstd], W=[nmr])
                    op("act", lambda e: e.activation(out=XH[:], in_=X[:], func=AF.Identity, bias=nmr[:], scale=rstd[:]),
                       R=[X, nmr, rstd], W=[XH])
                    if 'rot' not in KSKIP:
                        P = posf[kind]
                        op("dve", lambda e: e.tensor_scalar(out=tu[:], in0=invf[:], scalar1=P[:, idx:idx + 1], scalar2=None,
                                                            op0=OP.mult), R=[invf, P], W=[tu])
                        op("dve", lambda e: e.tensor_copy(out=tk[:], in_=tu[:]), R=[tu], W=[tk])
                        op("dve", lambda e: e.tensor_tensor(out=tf[:], in0=tu[:], in1=tk[:], op=OP.subtract), R=[tu, tk], W=[tf])
                        op("dve", lambda e: e.scalar_tensor_tensor(out=tg[:], in0=tf[:], scalar=0.5, in1=tf[:], op0=OP.is_gt,
                                                                   op1=OP.subtract), R=[tf], W=[tg])
                        op("act", lambda e: e.activation(out=TB[:, 0, :], in_=tg[:], func=AF.Sin, scale=-2.0 * math.pi),
                           R=[tg], W=[TB])
                        op("dve", lambda e: e.tensor_scalar(out=tf[:], in0=tf[:], scalar1=0.25, scalar2=None, op0=OP.add),
                           R=[tf], W=[tf])
                        op("dve", lambda e: e.scalar_tensor_tensor(out=tg[:], in0=tf[:], scalar=0.5, in1=tf[:], op0=OP.is_gt,
                                                                   op1=OP.subtract), R=[tf], W=[tg])
                        op("act", lambda e: e.activation(out=TB[:, 1, :], in_=tg[:], func=AF.Sin, scale=-2.0 * math.pi),
                           R=[tg], W=[TB])
                        if pass_id == 1:
                            op("dve", lambda e: e.tensor_scalar(out=TB[:, :, 0:128], in0=TB[:, :, 0:128], scalar1=1.0 / 16.0,
                                                                scalar2=None, op0=OP.mult), R=[TB], W=[TB])
                        else:
                            op("dve", lambda e: e.tensor_scalar(out=TB[:, :, 128:144], in0=TB[:, :, 128:144],
                                                                scalar1=128.0 ** -0.5, scalar2=None, op0=OP.mult), R=[TB], W=[TB])
                            XN = X
                            op("pool", lambda e: e.tensor_tensor(out=XN[:], in0=XH[:], in1=g0[:], op=OP.mult), R=[XH, g0], W=[XN])
                            op("pool", lambda e: e.tensor_tensor(out=XN[:], in0=XN[:], in1=b0[:], op=OP.add), R=[XN, b0], W=[XN])
                            dma("pool", S["xn"][idx * 128:(idx + 1) * 128, :], XN[:], R=[XN])
                elif part == 3:
                    if True:
                        for g in range(4):
                            p = pT[g % 2]
                            for j in range(4):
                                c = g * 4 + j
                                op("pe", lambda e, c=c, j=j, p=p: e.transpose(out=p[:, j * 128:(j + 1) * 128],
                                                                              in_=XH[:, c * 128:(c + 1) * 128],
                                                                              identity=ident_f[:]), R=[XH, ident_f], W=[p])
                            for j in range(4):
                                c = g * 4 + j
                                if g % 2 == 0:
                                    op("act", lambda e, c=c, j=j, p=p: e.activation(out=HT[:, c, :], in_=p[:, j * 128:(j + 1) * 128],
                                                                                    func=AF.Identity, bias=cols[:, 1, c:c + 1],
                                                                                    scale=cols[:, 0, c:c + 1]),
                                       R=[p, cols], W=[HT])
                                else:
                                    op("dve", lambda e, c=c, j=j, p=p: e.tensor_scalar(out=HT[:, c, :], in0=p[:, j * 128:(j + 1) * 128],
                                                                                       scalar1=cols[:, 0, c:c + 1],
                                                                                       scalar2=cols[:, 1, c:c + 1],
                                                                                       op0=OP.mult, op1=OP.add),
                                       R=[p, cols], W=[HT])
                else:
                    if 'mm' not in KSKIP:
                        if pass_id == 1:
                            halo = (kind == "oth" and idx < NHALO)
                            slices = list(range(8)) if (kind == "own" or halo) else [4, 5, 6, 7]
                        else:
                            slices = list(range(6))
                        for s in slices:
                            if hook is not None and s == slices[-2]:
                                hook()
                            p = pz[zc["n"] % NPZ]
                            zc["n"] += 1
                            for kc in range(KC):
                                op("pe", lambda e, kc=kc, p=p, s=s: e.matmul(p[:], lhsT=HT[:, kc, :], rhs=W[:, kc, s * 512:(s + 1) * 512],
                                                                             start=(kc == 0), stop=(kc == KC - 1)),
                                   R=[HT, W], W=[p])
                            zs = ZO[:, s * 512:(s + 1) * 512]
                            if pass_id == 1:
                                typ = ("arot", "arot", "copy", "copy", "rrot", "rrot", "copy", "copy")[s]
                            else:
                                typ = ("aqrot", "aqrot", "rrot", "rrot", "silu", "silu")[s]
                            if 'post' in KSKIP:
                                continue
                            if 'rotp' in KSKIP and typ not in ("copy", "silu"):
                                typ = "copy"
                            if 'rota' in KSKIP and typ in ("arot", "aqrot"):
                                typ = "copy"
                            if 'rotr' in KSKIP and typ == "rrot":
                                typ = "copy"
                            if 'pool' in KSKIP and typ not in ("copy", "silu"):
                                typ = typ + "_nopool"
                            if typ == "copy":
                                op("act", lambda e, p=p, zs=zs: e.copy(out=zs, in_=p[:]), R=[p], W=[ZO])
                            elif typ == "silu":
                                op("act", lambda e, p=p, zs=zs: e.activation(out=zs, in_=p[:], func=AF.Silu), R=[p], W=[ZO])
                            elif typ.startswith("a"):
                                sc = 1.0 if typ.startswith("arot") else 128.0 ** -0.5
                                op("act", lambda e, p=p, zs=zs, sc=sc: e.activation(out=zs, in_=p[:], func=AF.Identity, scale=sc),
                                   R=[p], W=[ZO])
                                pv = p[:].rearrange("p (h d) -> p h d", d=128)
                                zv = zs.rearrange("p (h d) -> p h d", d=128)
                                Sn = TB[:, 0, 128:144].unsqueeze(1).to_broadcast([128, 4, 16])
                                Cs = TB[:, 1, 128:144].unsqueeze(1).to_broadcast([128, 4, 16])
                                x1 = pv[:, :, 0:16]
                                x2 = pv[:, :, 16:32]
                                a, b2, c2, d2 = [rt[i][:, 0:64].rearrange("p (h d) -> p h d", d=16) for i in range(4)]
                                op("dve", lambda e, x1=x1, Cs=Cs, a=a: e.tensor_tensor(out=a, in0=x1, in1=Cs, op=OP.mult), R=[p, TB], W=[rt[0]])
                                op("dve", lambda e, x2=x2, Sn=Sn, b2=b2: e.tensor_tensor(out=b2, in0=x2, in1=Sn, op=OP.mult), R=[p, TB], W=[rt[1]])
                                op("dve", lambda e, x2=x2, Cs=Cs, c2=c2: e.tensor_tensor(out=c2, in0=x2, in1=Cs, op=OP.mult), R=[p, TB], W=[rt[2]])
                                op("dve", lambda e, x1=x1, Sn=Sn, d2=d2: e.tensor_tensor(out=d2, in0=x1, in1=Sn, op=OP.mult), R=[p, TB], W=[rt[3]])
                                op(("dve" if "pool" in KSKIP else "pool"), lambda e, zv=zv, a=a, b2=b2: e.tensor_tensor(out=zv[:, :, 0:16], in0=a, in1=b2, op=OP.subtract),
                                   R=[rt[0], rt[1]], W=[ZO])
                                op(("dve" if "pool" in KSKIP else "pool"), lambda e, zv=zv, c2=c2, d2=d2: e.tensor_tensor(out=zv[:, :, 16:32], in0=c2, in1=d2, op=OP.add),
                                   R=[rt[2], rt[3]], W=[ZO])
                            else:
                                pv = p[:].rearrange("p (h t d) -> p h t d", t=2, d=128)
                                zv = zs.rearrange("p (h t d) -> p h t d", t=2, d=128)
                                Sn = TB[:, 0, 0:128].unsqueeze(1).to_broadcast([128, 2, 128])
                                Cs = TB[:, 1, 0:128].unsqueeze(1).to_broadcast([128, 2, 128])
                                x1 = pv[:, :, 0, :]
                                x2 = pv[:, :, 1, :]
                                a, b2, c2, d2 = [rt[i][:].rearrange("p (h d) -> p h d", d=128) for i in range(4)]
                                op("dve", lambda e, x1=x1, Cs=Cs, a=a: e.tensor_tensor(out=a, in0=x1, in1=Cs, op=OP.mult), R=[p, TB], W=[rt[0]])
                                op("dve", lambda e, x2=x2, Sn=Sn, b2=b2: e.tensor_tensor(out=b2, in0=x2, in1=Sn, op=OP.mult), R=[p, TB], W=[rt[1]])
                                op("dve", lambda e, x2=x2, Cs=Cs, c2=c2: e.tensor_tensor(out=c2, in0=x2, in1=Cs, op=OP.mult), R=[p, TB], W=[rt[2]])
                                op("dve", lambda e, x1=x1, Sn=Sn, d2=d2: e.tensor_tensor(out=d2, in0=x1, in1=Sn, op=OP.mult), R=[p, TB], W=[rt[3]])
                                op(("dve" if "pool" in KSKIP else "pool"), lambda e, zv=zv, a=a, b2=b2: e.tensor_tensor(out=zv[:, :, 0, :], in0=a, in1=b2, op=OP.subtract),
                                   R=[rt[0], rt[1]], W=[ZO])
                                op(("dve" if "pool" in KSKIP else "pool"), lambda e, zv=zv, c2=c2, d2=d2: e.tensor_tensor(out=zv[:, :, 1, :], in0=c2, in1=d2, op=OP.add),
                                   R=[rt[2], rt[3]], W=[ZO])
                    if 'out' not in KSKIP:
                        r0 = idx * 128
                        if pass_id == 1:
                            if kind == "own":
                                a0 = 1024 + r0
                                dma("pool", S["ak"][a0:a0 + 128, :], ZO[:, 0:1024], R=[ZO])
                                dma("pool", S["av"][a0:a0 + 128, :], ZO[:, 1024:2048], R=[ZO])
                                dma("pool", S["rk"][r0:r0 + 128, :], ZO[:, 2048:3072], R=[ZO])
                                dma("pool", S["rv"][r0:r0 + 128, :], ZO[:, 3072:4096], R=[ZO])
                            else:
                                if idx < NHALO:
                                    for a0 in (r0, 1024 + SL + r0):
                                        dma("pool", S["ak"][a0:a0 + 128, :], ZO[:, 0:1024], R=[ZO])
                                        dma("pool", S["av"][a0:a0 + 128, :], ZO[:, 1024:2048], R=[ZO])
                                KW = kw[ti % 2]
                                for h in range(4):
                                    op("pool", lambda e, h=h, KW=KW: e.tensor_scalar(out=KW[:, h * 256:(h + 1) * 256],
                                                                                     in0=ZO[:, 2048 + h * 256: 2048 + (h + 1) * 256],
                                                                                     scalar1=wo[:, idx, h:h + 1], scalar2=0.0,
                                                                                     op0=OP.mult, op1=OP.add), R=[ZO, wo], W=[KW])
                                for h in range(4):
                                    for dc in range(2):
                                        op("pe", lambda e, h=h, dc=dc, KW=KW: e.matmul(
                                            pS[h][:, dc * 256:(dc + 1) * 256],
                                            lhsT=KW[:, h * 256 + dc * 128: h * 256 + (dc + 1) * 128],
                                            rhs=ZO[:, 3072 + h * 256: 3072 + (h + 1) * 256],
                                            start=False, stop=False, skip_group_check=True), R=[KW, ZO], W=[pS[h]])
                                if idx == NO - 1:
                                    for h in range(4):
                                        op("pe", lambda e, h=h: e.matmul(pS[h][:], lhsT=zer[:], rhs=W[:, 0, 0:512], start=False,
                                                                         stop=True, skip_group_check=True), R=[zer, W], W=[pS[h]])
                                    for h in range(4):
                                        for dc in range(2):
                                            rb = rt[(2 * h + dc) % 4]
                                            op("act", lambda e, h=h, dc=dc, rb=rb: e.copy(out=rb[:], in_=pS[h][:, dc * 256:(dc + 1) * 256]),
                                               R=[pS[h]], W=[rb])
                                            dma("pool", S["sin"][h, dc], rb[:], R=[rb])
                        else:
                            dma("pool", S["aq"][r0:r0 + 128, :], ZO[:, 0:1024], R=[ZO])
                            dma("pool", S["rq"][r0:r0 + 128, :], ZO[:, 1024:2048], R=[ZO])
                            dma("pool", S["rg"][r0:r0 + 128, :], ZO[:, 2048:3072], R=[ZO])

            stage(0, 0)
            if len(tiles) > 1:
                stage(1, 0)
            stage(0, 1)
            stage(0, 3)
            for ti_ in range(len(tiles)):
                if ti_ + 2 < len(tiles):
                    stage(ti_ + 2, 0)
                hk = None
                if ti_ + 1 < len(tiles):
                    stage(ti_ + 1, 1)
                    hk = (lambda t=ti_: stage(t + 1, 3))
                stage(ti_, 2, hook=hk)


    LN_EPS_B = k.sb("lneps", [128, 1], F32)
    LN_EPS_AP = LN_EPS_B
    op("dve", lambda e: e.memset(LN_EPS_B[:], LN_EPS), W=[LN_EPS_B])

    phaseA(1)
    phaseA(2)
    if stop_after == "A":
        k.close()
        return nc

    S["ao"] = [dscr("ao%d" % p, [SL, 8, 132], F32) for p in range(3)]
    with k.scope():
        negm = k.sb("negm", [128, 4, 128], BF16)
        dma("sp", negm[:], I["negmask"], W=[negm])
        krow = [k.sb("krow%d" % i, [128, 1024], BF16) for i in range(2)]
        qrow = [k.sb("qrow%d" % i, [128, 1024], BF16) for i in range(2)]
        KT = [k.sb("KT%d" % i, [128, 8, 128], BF16) for i in range(3)]
        QT = [k.sb("QT%d" % i, [128, 8, 128], BF16) for i in range(2)]
        VX = [k.sb("VX%d" % i, [128, 8, 132], BF16) for i in range(3)]
        for v in VX:
            op("dve", lambda e, v=v: e.memset(v[:, :, 128:129], 1.0), W=[v])
        pt = [k.sb("pt%d" % i, [128, 256], BF16) for i in range(2)]
        osb = [k.sb("osb%d" % i, [128, 8, 132], F32) for i in range(2)]
        for o_ in osb:
            op("dve", lambda e, o_=o_: e.memset(o_[:], 0.0), W=[o_])
        ptr = [k.ps("ptr%d" % i, [128, 1024], BF16) for i in range(2)]
        pss = [k.ps("pss%d" % i, [128, 512], F32) for i in range(2)]
        pso = [k.ps("pso%d" % i, [128, 512], F32) for i in range(2)]
        cnt = {"tr": 0, "s": 0, "kl": 0, "ql": 0, "q": 0}

        def load_T(rows_ap, rowbuf, dstT, eng):
            dma("sp", rowbuf[:], rows_ap, W=[rowbuf])
            p = ptr[cnt["tr"] % 2]
            cnt["tr"] += 1
            for h in range(8):
                op("pe", lambda e, h=h, p=p: e.transpose(out=p[:, h * 128:(h + 1) * 128], in_=rowbuf[:, h * 128:(h + 1) * 128],
                                                         identity=ident_b[:]), R=[rowbuf, ident_b], W=[p])
            if eng == "act":
                op("act", lambda e, p=p: e.copy(out=dstT[:].rearrange("p h d -> p (h d)"), in_=p[:]), R=[p], W=[dstT])
            else:
                op("dve", lambda e, p=p: e.tensor_copy(out=dstT[:].rearrange("p h d -> p (h d)"), in_=p[:]), R=[p], W=[dstT])

        for pi, dil in enumerate((1, 4, 16)):
            akv = S["ak"].rearrange("(n d) c -> d n c", d=dil)
            avv = S["av"].rearrange("(n d) c -> d n c", d=dil)
            aqv = S["aq"].rearrange("(n d) c -> d n c", d=dil)
            aov = S["ao"][pi].rearrange("(n d) h c -> d n h c", d=dil)
            NQ = SL // dil // 128
            n_base = 1024 // dil - 64
            for r in range(dil):
                def load_k(j):
                    n0 = n_base + 128 * j
                    slot = j % 3
                    load_T(akv[r, n0:n0 + 128, :], krow[cnt["kl"] % 2], KT[slot], "dve")
                    cnt["kl"] += 1
                    dma("sp", VX[slot][:, :, 0:128], avv[r, n0:n0 + 128, :].rearrange("n (h d) -> n h d", d=128), W=[VX[slot]])
                load_k(0)
                for i in range(NQ):
                    load_k(i + 1)
                    Q = QT[i % 2]
                    load_T(aqv[r, 128 * i:128 * (i + 1), :], qrow[cnt["ql"] % 2], Q, "dve")
                    cnt["ql"] += 1
                    O = osb[cnt["q"] % 2]
                    cnt["q"] += 1
                    mA = 2 if i == 0 else 0
                    mB = 3 if i == NQ - 1 else 1
                    def scores(h):
                        ps = pss[cnt["s"] % 2]
                        po = pso[cnt["s"] % 2]
                        P = pt[cnt["s"] % 2]
                        cnt["s"] += 1
                        for half, (slot, mi) in enumerate((((i) % 3, mA), ((i + 1) % 3, mB))):
                            dst = ps[:, half * 128:(half + 1) * 128]
                            op("pe", lambda e, dst=dst, slot=slot, h=h, Q=Q: e.matmul(dst, lhsT=KT[slot][:, h, :], rhs=Q[:, h, :],
                                                                                     start=True, stop=False, skip_group_check=True),
                               R=[KT[slot], Q], W=[ps])
                            op("pe", lambda e, dst=dst, mi=mi: e.matmul(dst, lhsT=ident_b[:], rhs=negm[:, mi, :], start=False,
                                                                        stop=True, skip_group_check=True),
                               R=[ident_b, negm], W=[ps])
                        op("act", lambda e, ps=ps, P=P: e.activation(out=P[:], in_=ps[:, 0:256], func=AF.Exp), R=[ps], W=[P])
                        return (h, po, P)

                    def pvs(hpp):
                        h, po, P = hpp
                        for half, slot in enumerate(((i) % 3, (i + 1) % 3)):
                            op("pe", lambda e, half=half, slot=slot, h=h, po=po, P=P: e.matmul(
                                po[:, 0:129], lhsT=P[:, half * 128:(half + 1) * 128], rhs=VX[slot][:, h, 0:129],
                                start=(half == 0), stop=(half == 1)), R=[P, VX[slot]], W=[po])
                        op("dve", lambda e, h=h, po=po, O=O: e.tensor_copy(out=O[:, h, 0:129], in_=po[:, 0:129]), R=[po], W=[O])

                    prev = None
                    for h in range(8):
                        cur = scores(h)
                        if prev is not None:
                            pvs(prev)
                        prev = cur
                    pvs(prev)
                    dma("pool", aov[r, 128 * i:128 * (i + 1), :, :], O[:], R=[O])
    if stop_after == "B1":
        k.close()
        return nc

    S["sb"] = dscr("sbst", [NT, 128, 4, 512], BF16)
    S["ret"] = dscr("ret", [SL, 1024], BF16)
    with k.scope():
        lgf = k.sb("lgf", [128, 4], F32)
        lgb = k.sb("lgb", [128, 4], F32)
        bc_load("sp", lgf, I["lg_f"])
        bc_load("sp", lgb, I["lg_b"])
        cst = {}
        for n in ("dpos", "dneg", "mfw", "mbw", "idxrow"):
            cst[n] = k.sb(n, [128, 128], F32)
            dma("sp", cst[n][:], I[n], W=[cst[n]])
        idxc = k.sb("idxc", [128, 1], F32)
        dma("sp", idxc[:], I["idxcol"], W=[idxc])
        flg = k.sb("flg", [128, 2], F32)
        dma("sp", flg[:], I["flags"], W=[flg])
        DmT = k.sb("DmT", [128, 4, 128], F32)
        xif = k.sb("xif", [128, 4, 128], BF16)
        xib = k.sb("xib", [128, 4, 128], BF16)
        zf = k.sb("zf", [128, 4], F32)
        zb = k.sb("zb", [128, 4], F32)
        cdf = k.sb("cdf", [128, 4], F32)
        cdb = k.sb("cdb", [128, 4], F32)
        tmpa = k.sb("tmpa", [128, 128], F32)
        tmpb = k.sb("tmpb", [128, 128], F32)
        sc = k.sb("sc", [128, 8], F32)
        zero1 = k.sb("zero1", [128, 1], F32)
        op("dve", lambda e: e.memset(zero1[:], 0.0), W=[zero1])
        for h in range(4):
            op("act", lambda e, h=h: e.activation(out=tmpa[:], in_=cst["dpos"][:], func=AF.Exp, scale=lgf[:, h:h + 1]),
               R=[cst["dpos"], lgf], W=[tmpa])
            op("dve", lambda e: e.tensor_tensor(out=tmpa[:], in0=tmpa[:], in1=cst["mfw"][:], op=OP.mult), R=[tmpa, cst["mfw"]], W=[tmpa])
            op("act", lambda e, h=h: e.activation(out=tmpb[:], in_=cst["dneg"][:], func=AF.Exp, scale=lgb[:, h:h + 1]),
               R=[cst["dneg"], lgb], W=[tmpb])
            op("dve", lambda e: e.tensor_tensor(out=tmpb[:], in0=tmpb[:], in1=cst["mbw"][:], op=OP.mult), R=[tmpb, cst["mbw"]], W=[tmpb])
            op("dve", lambda e, h=h: e.tensor_tensor(out=DmT[:, h, :], in0=tmpa[:], in1=tmpb[:], op=OP.add), R=[tmpa, tmpb], W=[DmT])
            op("act", lambda e, h=h: e.activation(out=xif[:, h, :], in_=cst["idxrow"][:], func=AF.Exp, scale=lgf[:, h:h + 1],
                                                  bias=lgf[:, h:h + 1]), R=[cst["idxrow"], lgf], W=[xif])
            op("dve", lambda e, h=h: e.tensor_scalar(out=sc[:, 0:1], in0=lgb[:, h:h + 1], scalar1=-1.0, scalar2=None, op0=OP.mult),
               R=[lgb], W=[sc])
            op("dve", lambda e, h=h: e.tensor_scalar(out=sc[:, 1:2], in0=lgb[:, h:h + 1], scalar1=128.0, scalar2=None, op0=OP.mult),
               R=[lgb], W=[sc])
            op("act", lambda e, h=h: e.activation(out=xib[:, h, :], in_=cst["idxrow"][:], func=AF.Exp, scale=sc[:, 0:1],
                                                  bias=sc[:, 1:2]), R=[cst["idxrow"], sc], W=[xib])
            op("dve", lambda e, h=h: e.tensor_scalar(out=sc[:, 2:3], in0=lgf[:, h:h + 1], scalar1=-1.0, scalar2=None, op0=OP.mult),
               R=[lgf], W=[sc])
            op("dve", lambda e, h=h: e.tensor_scalar(out=sc[:, 3:4], in0=lgf[:, h:h + 1], scalar1=127.0, scalar2=None, op0=OP.mult),
               R=[lgf], W=[sc])
            op("act", lambda e, h=h: e.activation(out=zf[:, h:h + 1], in_=idxc[:], func=AF.Exp, scale=sc[:, 2:3], bias=sc[:, 3:4]),
               R=[idxc, sc], W=[zf])
            op("act", lambda e, h=h: e.activation(out=zb[:, h:h + 1], in_=idxc[:], func=AF.Exp, scale=lgb[:, h:h + 1], bias=zero1[:]),
               R=[idxc, lgb, zero1], W=[zb])
        op("act", lambda e: e.activation(out=cdf[:], in_=lgf[:], func=AF.Exp, scale=128.0), R=[lgf], W=[cdf])
        op("act", lambda e: e.activation(out=cdb[:], in_=lgb[:], func=AF.Exp, scale=128.0), R=[lgb], W=[cdb])
        Sin = k.sb("Sin", [128, 4, 512], F32)
        dma("sp", Sin[:].rearrange("p h (c n) -> p h c n", c=2), S["sin"].rearrange("h c p n -> p h c n"), W=[Sin])
        Sf = k.sb("Sf", [128, 4, 512], F32)
        Sb = k.sb("Sb", [128, 4, 512], F32)
        op("dve", lambda e: e.tensor_scalar(out=Sf[:], in0=Sin[:], scalar1=flg[:, 0:1], scalar2=None, op0=OP.mult), R=[Sin, flg], W=[Sf])
        op("dve", lambda e: e.tensor_scalar(out=Sb[:], in0=Sin[:], scalar1=flg[:, 1:2], scalar2=None, op0=OP.mult), R=[Sin, flg], W=[Sb])
        Sbf = [k.sb("Sbf%d" % i, [128, 4, 512], BF16) for i in range(2)]
        Sff = [k.sb("Sff%d" % i, [128, 4, 512], BF16) for i in range(2)]
        kt_ = [k.sb("rk%d" % i, [128, 1024], BF16) for i in range(2)]
        vt_ = [k.sb("rv%d" % i, [128, 1024], BF16) for i in range(2)]
        qt_ = [k.sb("rq%d" % i, [128, 1024], BF16) for i in range(2)]
        gt_ = [k.sb("rg%d" % i, [128, 1024], BF16) for i in range(2)]
        kw_ = [k.sb("kwr%d" % i, [128, 1024], BF16) for i in range(2)]
        pS = [k.ps("pS%d" % i, [128, 512], F32) for i in range(2)]
        for ci, c in enumerate(range(NT - 1, -1, -1)):
            SB = Sbf[ci % 2]
            op("act", lambda e, SB=SB: e.copy(out=SB[:], in_=Sb[:]), R=[Sb], W=[SB])
            dma("pool", S["sb"][c], SB[:], R=[SB])
            if c == 0:
                break
            Kt = kt_[ci % 2]
            Vt = vt_[ci % 2]
            KW = kw_[ci % 2]
            dma("sp", Kt[:], S["rk"][c * 128:(c + 1) * 128, :], W=[Kt])
            dma("sp", Vt[:], S["rv"][c * 128:(c + 1) * 128, :], W=[Vt])
            for h in range(4):
                op("pool", lambda e, h=h, KW=KW, Kt=Kt: e.tensor_scalar(out=KW[:, h * 256:(h + 1) * 256], in0=Kt[:, h * 256:(h + 1) * 256],
                                                                        scalar1=zb[:, h:h + 1], scalar2=0.0, op0=OP.mult, op1=OP.add),
                   R=[Kt, zb], W=[KW])
            for h in range(4):
                p = pS[h % 2]
                for dc in range(2):
                    op("pe", lambda e, h=h, dc=dc, p=p, KW=KW, Vt=Vt: e.matmul(
                        p[:, dc * 256:(dc + 1) * 256], lhsT=KW[:, h * 256 + dc * 128:h * 256 + (dc + 1) * 128],
                        rhs=Vt[:, h * 256:(h + 1) * 256], start=True, stop=True, skip_group_check=True), R=[KW, Vt], W=[p])
                op("dve", lambda e, h=h, p=p: e.scalar_tensor_tensor(out=Sb[:, h, :], in0=Sb[:, h, :], scalar=cdb[:, h:h + 1], in1=p[:],
                                                                     op0=OP.mult, op1=OP.add), R=[Sb, cdb, p], W=[Sb])
        k.fence()
        QT2 = [k.sb("QT2%d" % i, [128, 8, 128], BF16) for i in range(2)]
        KT2 = [k.sb("KT2%d" % i, [128, 8, 128], BF16) for i in range(2)]
        Qf = [k.sb("Qf%d" % i, [128, 2, 128], BF16) for i in range(2)]
        Qb = [k.sb("Qb%d" % i, [128, 2, 128], BF16) for i in range(2)]
        PT = [k.sb("PT%d" % i, [128, 128], BF16) for i in range(2)]
        ynm = [k.sb("ynm%d" % i, [128, 256], F32) for i in range(2)]
        ro = [k.sb("ro%d" % i, [128, 1024], BF16) for i in range(2)]
        st2 = k.sb("st2", [128, 6], F32)
        mv2 = k.sb("mv2", [128, 2], F32)
        rs2 = k.sb("rs2", [128, 1], F32)
        nm2 = k.sb("nm2", [128, 1], F32)
        ptq = k.ps("ptq", [128, 1024], BF16)
        ptk = k.ps("ptk", [128, 1024], BF16)
        pa = [k.ps("pa%d" % i, [128, 512], F32) for i in range(2)]
        py = [k.ps("py%d" % i, [128, 512], F32) for i in range(2)]
        SFb = Sff[0]
        op("act", lambda e: e.copy(out=SFb[:], in_=Sf[:]), R=[Sf], W=[SFb])
        hc = 0
        for c in range(NT):
            Kt, Vt, Qt, Gt, KW, SB = kt_[c % 2], vt_[c % 2], qt_[c % 2], gt_[c % 2], kw_[c % 2], Sbf[c % 2]
            RO = ro[c % 2]
            rows = slice(c * 128, (c + 1) * 128)
            dma("sp", Kt[:], S["rk"][rows, :], W=[Kt])
            dma("sp", Vt[:], S["rv"][rows, :], W=[Vt])
            dma("sp", Qt[:], S["rq"][rows, :], W=[Qt])
            dma("sp", Gt[:], S["rg"][rows, :], W=[Gt])
            dma("sp", SB[:], S["sb"][c], W=[SB])
            QT_, KT_ = QT2[c % 2], KT2[c % 2]
            for (src, p, dst, eng) in ((Qt, ptq, QT_, "act"), (Kt, ptk, KT_, "dve")):
                for j in range(8):
                    op("pe", lambda e, j=j, p=p, src=src: e.transpose(out=p[:, j * 128:(j + 1) * 128], in_=src[:, j * 128:(j + 1) * 128],
                                                                      identity=ident_b[:]), R=[src, ident_b], W=[p])
                if eng == "act":
                    op("act", lambda e, p=p, dst=dst: e.copy(out=dst[:].rearrange("p h d -> p (h d)"), in_=p[:]), R=[p], W=[dst])
                else:
                    op("dve", lambda e, p=p, dst=dst: e.tensor_copy(out=dst[:].rearrange("p h d -> p (h d)"), in_=p[:]), R=[p], W=[dst])
            for h in range(4):
                op("pool", lambda e, h=h, KW=KW, Kt=Kt: e.tensor_scalar(out=KW[:, h * 256:(h + 1) * 256], in0=Kt[:, h * 256:(h + 1) * 256],
                                                                        scalar1=zf[:, h:h + 1], scalar2=0.0, op0=OP.mult, op1=OP.add),
                   R=[Kt, zf], W=[KW])
            SFn = Sff[(c + 1) % 2]
            def partA(h, hc):
                A = pa[hc % 2]
                Y = py[hc % 2]
                P_ = PT[hc % 2]
                QF, QB = Qf[hc % 2], Qb[hc % 2]
                YN = ynm[hc % 2]
                for dc in range(2):
                    op("pe", lambda e, h=h, dc=dc, A=A: e.matmul(A[:, 0:128], lhsT=KT_[:, 2 * h + dc, :], rhs=QT_[:, 2 * h + dc, :],
                                                                 start=(dc == 0), stop=(dc == 1)), R=[KT_, QT_], W=[A])
                op("dve", lambda e, h=h, A=A, P_=P_: e.tensor_tensor(out=P_[:], in0=A[:, 0:128], in1=DmT[:, h, :], op=OP.mult),
                   R=[A, DmT], W=[P_])
                op("pool", lambda e, h=h, QF=QF: e.tensor_tensor(out=QF[:], in0=QT_[:, 2 * h:2 * h + 2, :],
                                                                 in1=xif[:, h, :].unsqueeze(1).to_broadcast([128, 2, 128]), op=OP.mult),
                   R=[QT_, xif], W=[QF])
                op("pool", lambda e, h=h, QB=QB: e.tensor_tensor(out=QB[:], in0=QT_[:, 2 * h:2 * h + 2, :],
                                                                 in1=xib[:, h, :].unsqueeze(1).to_broadcast([128, 2, 128]), op=OP.mult),
                   R=[QT_, xib], W=[QB])
                return (h, A, Y, P_, QF, QB, YN)

            def partB(bufs):
                h, A, Y, P_, QF, QB, YN = bufs
                yv = Y[:, 0:256]
                op("pe", lambda e, h=h, yv=yv, P_=P_: e.matmul(yv, lhsT=P_[:], rhs=Vt[:, h * 256:(h + 1) * 256], start=True, stop=False),
                   R=[P_, Vt], W=[Y])
                for dc in range(2):
                    op("pe", lambda e, h=h, dc=dc, yv=yv, QF=QF: e.matmul(yv, lhsT=QF[:, dc, :], rhs=SFb[:, h, dc * 256:(dc + 1) * 256],
                                                                          start=False, stop=False), R=[QF, SFb], W=[Y])
                for dc in range(2):
                    op("pe", lambda e, h=h, dc=dc, yv=yv, QB=QB: e.matmul(yv, lhsT=QB[:, dc, :], rhs=SB[:, h, dc * 256:(dc + 1) * 256],
                                                                          start=False, stop=(dc == 1)), R=[QB, SB], W=[Y])
                p = pS[h % 2]
                for dc in range(2):
                    op("pe", lambda e, h=h, dc=dc, p=p: e.matmul(
                        p[:, dc * 256:(dc + 1) * 256], lhsT=KW[:, h * 256 + dc * 128:h * 256 + (dc + 1) * 128],
                        rhs=Vt[:, h * 256:(h + 1) * 256], start=True, stop=True, skip_group_check=True), R=[KW, Vt], W=[p])
                op("dve", lambda e, h=h, p=p: e.scalar_tensor_tensor(out=Sf[:, h, :], in0=Sf[:, h, :], scalar=cdf[:, h:h + 1], in1=p[:],
                                                                     op0=OP.mult, op1=OP.add), R=[Sf, cdf, p], W=[Sf])
                op("act", lambda e, h=h, SFn=SFn: e.copy(out=SFn[:, h, :], in_=Sf[:, h, :]), R=[Sf], W=[SFn])
                op("dve", lambda e, yv=yv: e.bn_stats(out=st2[:], in_=yv), R=[Y], W=[st2])
                op("dve", lambda e: e.bn_aggr(out=mv2[:], in_=st2[:]), R=[st2], W=[mv2])
                op("act", lambda e: e.activation(out=rs2[:], in_=mv2[:, 1:2], func=AF.Sqrt, bias=LN_EPS_AP[:], scale=1.0),
                   R=[mv2, LN_EPS_B], W=[rs2])
                op("dve", lambda e: e.reciprocal(out=rs2[:], in_=rs2[:]), R=[rs2], W=[rs2])
                op("dve", lambda e: e.tensor_scalar(out=nm2[:], in0=mv2[:, 0:1], scalar1=rs2[:], scalar2=-1.0, op0=OP.mult, op1=OP.mult),
                   R=[mv2, rs2], W=[nm2])
                op("act", lambda e, yv=yv, YN=YN: e.activation(out=YN[:], in_=yv, func=AF.Identity, bias=nm2[:], scale=rs2[:]),
                   R=[Y, nm2, rs2], W=[YN])
                op("pool", lambda e, h=h, YN=YN, RO=RO, Gt=Gt: e.tensor_tensor(out=RO[:, h * 256:(h + 1) * 256], in0=YN[:],
                                                                             in1=Gt[:, h * 256:(h + 1) * 256], op=OP.mult),
                   R=[YN, Gt], W=[RO])

            prevb = None
            for h in range(4):
                curb = partA(h, hc)
                hc += 1
                if prevb is not None:
                    partB(prevb)
                prevb = curb
            partB(prevb)
            SFb = SFn
            dma("pool", S["ret"][rows, :], RO[:], R=[RO])
    if stop_after == "B2":
        k.close()
        return nc

    S["x1"] = dscr("x1", [SL, D], F32)
    S["h2"] = dscr("h2", [SL, D], BF16)
    S["y"] = dscr("ymoe", [NSLOT, D], BF16)
    L_all = k.sb("L_all", [128, NT, 36], F32)
    DEST = k.sb("DEST", [128, NT, 2], I32)
    WT = k.sb("WT", [128, NT, 2], F32)
    if "rt" in dbg:
        S["rt"] = dscr("rt", [SL, 40], F32)
    with k.scope():
        Wo = k.sb("Wo", [128, KC, D], BF16)
        wout_v = I["w_out"].rearrange("(kc p) n -> p kc n", p=128)
        for j in range(4):
            dma("pool", Wo[:, :, j * 512:(j + 1) * 512], wout_v[:, :, j * 512:(j + 1) * 512], W=[Wo])
        with k.scope():
            r1 = k.sb("row_opg1", [128, D], F32)
            bc_load("sp", r1, S["vec"][VEC["opg1"]:VEC["opg1"] + 1, :])
            for kc in range(KC):
                op("pool" if kc % 2 else "dve", lambda e, kc=kc: e.tensor_tensor(out=Wo[:, kc, :], in0=Wo[:, kc, :], in1=r1[:], op=OP.mult),
                   R=[Wo, r1], W=[Wo])
        rows = {}
        for n in ("ln1_g", "ln1_b", "G2", "B2"):
            rows[n] = k.sb("row_" + n, [128, D], F32)
            bc_load("sp", rows[n], S["vec"][VEC[n]:VEC[n] + 1, :])
        Wr = k.sb("Wr", [128, KC, 36], F32)
        dma("sp", Wr[:], I["w_r"].rearrange("(kc p) n -> p kc n", p=128), W=[Wr])
        br = k.sb("br", [128, 36], F32)
        bc_load("sp", br, I["b_r"])
        eoff = k.sb("eoff", [128, 32], F32)
        dma("sp", eoff[:], I["iota_e"], W=[eoff])
        op("dve", lambda e: e.tensor_scalar(out=eoff[:], in0=eoff[:], scalar1=float(CAP), scalar2=None, op0=OP.mult), R=[eoff], W=[eoff])
        tokid = k.sb("tokid", [128, NT], I32)
        dma("sp", tokid[:], I["tokid"], W=[tokid])
        pcolN = k.sb("pcolN", [128, 1], F32)
        dma("sp", pcolN[:], I["idxcol"], W=[pcolN])
        op("dve", lambda e: e.tensor_scalar(out=pcolN[:], in0=pcolN[:], scalar1=float(NSLOT), scalar2=None, op0=OP.add), R=[pcolN], W=[pcolN])
        cntb = k.sb("cntb", [128, 32], F32)
        op("dve", lambda e: e.memset(cntb[:], 0.0), W=[cntb])
        ao_t = [k.sb("ao_t%d" % i, [128, 8, 132], F32) for i in range(6)]
        mix = [k.sb("mix%d" % i, [128, D], BF16) for i in range(2)]
        mixT = [k.sb("mixT%d" % i, [128, KC, 128], BF16) for i in range(2)]
        xr = [k.sb("xr%d" % i, [128, D], F32) for i in range(2)]
        xh1 = [k.sb("xh1_%d" % i, [128, D], F32) for i in range(2)]
        tf32 = k.sb("tf32", [128, D], F32)
        tb16 = k.sb("tb16", [128, D], BF16)
        h2T = k.sb("h2T", [128, KC, 128], F32)
        rden = k.sb("rden", [128, 8], F32)
        st = k.sb("stc", [128, 4, 6], F32)
        mv = k.sb("mvc", [128, 2], F32)
        rstd = k.sb("rstdc", [128, 1], F32)
        nmr = k.sb("nmrc", [128, 1], F32)
        L = k.sb("L", [128, 36], F32)
        r_ = {n: k.sb("r_" + n, shp, F32) for n, shp in (
            ("gmax", [128, 1]), ("ngmax", [128, 1]), ("gone", [128, 4]), ("ge", [128, 4]), ("gsum", [128, 1]), ("pg", [128, 1]),
            ("ss", [128, 8]), ("v0", [128, 1]), ("m0", [128, 8]), ("msk", [128, 8]), ("v1", [128, 1]), ("m1", [128, 8]),
            ("d10", [128, 1]), ("e1", [128, 1]), ("den", [128, 1]), ("M0", [128, 32]), ("M1", [128, 32]), ("M", [128, 32]),
            ("Rk", [128, 32]), ("t32", [128, 32]), ("rank", [128, 2]), ("eo", [128, 2]), ("valid", [128, 2]), ("dg", [128, 2]),
            ("ds", [128, 2]), ("w", [128, 2]))}
        Mb = k.sb("Mb", [128, 32], BF16)
        dsi = k.sb("dsi", [128, 2], I32)
        ptm = [k.ps("ptm%d" % i, [128, 1024], BF16) for i in range(2)]
        po_ = [k.ps("po%d" % i, [128, 512], F32) for i in range(2)]
        pth = [k.ps("pth%d" % i, [128, 512], F32) for i in range(2)]
        prt = k.ps("prt", [128, 512], F32)
        prk = k.ps("prk", [128, 512], F32)
        pcount = 0
        def c_loads_a(i):
            rws_ = slice(i * 128, (i + 1) * 128)
            for p in range(3):
                dma("sp", ao_t[(i % 2) * 3 + p][:], S["ao"][p][rws_, :, :], W=[ao_t[(i % 2) * 3 + p]])
            dma("sp", mix[i % 2][:, 1024:2048], S["ret"][rws_, :], W=[mix[i % 2]])

        def c_loads_x(i):
            dma("sp", xr[i % 2][:], S["xn"][i * 128:(i + 1) * 128, :], W=[xr[i % 2]])

        def c_front(i):
            MIX = mix[i % 2]
            AO = ao_t[(i % 2) * 3:(i % 2) * 3 + 3]
            MT = mixT[i % 2]
            op("dve", lambda e: e.tensor_tensor(out=AO[0][:], in0=AO[0][:], in1=AO[1][:], op=OP.add), R=[AO[0], AO[1]], W=[AO[0]])
            op("dve", lambda e: e.tensor_tensor(out=AO[0][:], in0=AO[0][:], in1=AO[2][:], op=OP.add), R=[AO[0], AO[2]], W=[AO[0]])
            op("dve", lambda e: e.tensor_copy(out=rden[:], in_=AO[0][:, :, 128]), R=[AO[0]], W=[rden])
            op("dve", lambda e: e.reciprocal(out=rden[:], in_=rden[:]), R=[rden], W=[rden])
            op("dve", lambda e, MIX=MIX: e.tensor_tensor(out=MIX[:, 0:1024].rearrange("p (h d) -> p h d", d=128), in0=AO[0][:, :, 0:128],
                                                         in1=rden[:].unsqueeze(2).to_broadcast([128, 8, 128]), op=OP.mult),
               R=[AO[0], rden], W=[MIX])
            for g in range(2):
                p = ptm[g]
                for j in range(8):
                    c = g * 8 + j
                    op("pe", lambda e, c=c, j=j, p=p, MIX=MIX: e.transpose(out=p[:, j * 128:(j + 1) * 128], in_=MIX[:, c * 128:(c + 1) * 128],
                                                                           identity=ident_b[:]), R=[MIX, ident_b], W=[p])
                dstv = MT[:, g * 8:(g + 1) * 8, :].rearrange("p c t -> p (c t)")
                if g == 0:
                    op("act", lambda e, p=p, dstv=dstv: e.copy(out=dstv, in_=p[:]), R=[p], W=[MT])
                else:
                    op("dve", lambda e, p=p, dstv=dstv: e.tensor_copy(out=dstv, in_=p[:]), R=[p], W=[MT])

        def c_mm(i):
            XR = xr[i % 2]
            MT_ = mixT[i % 2]
            for sl in range(4):
                p = po_[(i * 4 + sl) % 2]
                cs_ = slice(sl * 512, (sl + 1) * 512)
                for kc in range(KC):
                    op("pe", lambda e, kc=kc, p=p, cs_=cs_: e.matmul(p[:], lhsT=MT_[:, kc, :], rhs=Wo[:, kc, cs_], start=(kc == 0),
                                                                     stop=(kc == KC - 1)), R=[MT_, Wo], W=[p])
                op("dve", lambda e, p=p, cs_=cs_, XR=XR: e.scalar_tensor_tensor(out=XR[:, cs_], in0=XR[:, cs_], scalar=ALPHA, in1=p[:],
                                                                               op0=OP.mult, op1=OP.add), R=[XR, p], W=[XR])

        def c_ln(i):
            rws = slice(i * 128, (i + 1) * 128)
            XR = xr[i % 2]
            XH = xh1[i % 2]
            for c4 in range(4):
                op("dve", lambda e, c4=c4, XR=XR: e.bn_stats(out=st[:, c4, :], in_=XR[:, c4 * 512:(c4 + 1) * 512]), R=[XR], W=[st])
            op("dve", lambda e: e.bn_aggr(out=mv[:], in_=st[:].rearrange("p a b -> p (a b)")), R=[st], W=[mv])
            op("act", lambda e: e.activation(out=rstd[:], in_=mv[:, 1:2], func=AF.Sqrt, bias=LN_EPS_AP[:], scale=1.0), R=[mv, LN_EPS_B], W=[rstd])
            op("dve", lambda e: e.reciprocal(out=rstd[:], in_=rstd[:]), R=[rstd], W=[rstd])
            op("dve", lambda e: e.tensor_scalar(out=nmr[:], in0=mv[:, 0:1], scalar1=rstd[:], scalar2=-1.0, op0=OP.mult, op1=OP.mult),
               R=[mv, rstd], W=[nmr])
            op("act", lambda e, XR=XR: e.activation(out=XH[:], in_=XR[:], func=AF.Identity, bias=nmr[:], scale=rstd[:]), R=[XR, nmr, rstd], W=[XH])
            op("pool", lambda e: e.tensor_tensor(out=tf32[:], in0=XH[:], in1=rows["ln1_g"][:], op=OP.mult), R=[XH, rows["ln1_g"]], W=[tf32])
            op("pool", lambda e: e.tensor_tensor(out=tf32[:], in0=tf32[:], in1=rows["ln1_b"][:], op=OP.add), R=[tf32, rows["ln1_b"]], W=[tf32])
            dma("pool", S["x1"][rws, :], tf32[:], R=[tf32])
            op("pool", lambda e, XR=XR: e.tensor_tensor(out=XR[:], in0=XH[:], in1=rows["G2"][:], op=OP.mult), R=[XH, rows["G2"]], W=[XR])
            op("pool", lambda e, XR=XR: e.tensor_tensor(out=tb16[:], in0=XR[:], in1=rows["B2"][:], op=OP.add), R=[XR, rows["B2"]], W=[tb16])
            dma("pool", S["h2"][rws, :], tb16[:], R=[tb16])

        def c_router(i):
            XH = xh1[i % 2]
            for g in range(4):
                p = pth[g % 2]
                for j in range(4):
                    c = g * 4 + j
                    op("pe", lambda e, c=c, j=j, p=p: e.transpose(out=p[:, j * 128:(j + 1) * 128], in_=XH[:, c * 128:(c + 1) * 128],
                                                                  identity=ident_f[:]), R=[XH, ident_f], W=[p])
                for j in range(4):
                    c = g * 4 + j
                    if g % 2 == 0:
                        op("act", lambda e, c=c, j=j, p=p: e.activation(out=h2T[:, c, :], in_=p[:, j * 128:(j + 1) * 128], func=AF.Identity,
                                                                        bias=cols[:, 3, c:c + 1], scale=cols[:, 2, c:c + 1]), R=[p, cols], W=[h2T])
                    else:
                        op("dve", lambda e, c=c, j=j, p=p: e.tensor_scalar(out=h2T[:, c, :], in0=p[:, j * 128:(j + 1) * 128],
                                                                           scalar1=cols[:, 2, c:c + 1], scalar2=cols[:, 3, c:c + 1],
                                                                           op0=OP.mult, op1=OP.add), R=[p, cols], W=[h2T])
            for kc in range(KC):
                op("pe", lambda e, kc=kc: e.matmul(prt[:, 0:36], lhsT=h2T[:, kc, :], rhs=Wr[:, kc, :], start=(kc == 0), stop=(kc == KC - 1)),
                   R=[h2T, Wr], W=[prt])
            op("dve", lambda e: e.tensor_tensor(out=L_all[:, i, :], in0=prt[:, 0:36], in1=br[:], op=OP.add), R=[prt, br], W=[L_all])

        c_loads_a(0)
        c_loads_x(0)
        if NT > 1:
            c_loads_a(1)
        c_front(0)
        for i in range(NT):
            if i + 2 < NT:
                c_loads_a(i + 2)
            if i + 1 < NT:
                c_loads_x(i + 1)
            c_mm(i)
            if i + 1 < NT:
                c_front(i + 1)
            c_ln(i)
            if i >= 1:
                c_router(i - 1)
        c_router(NT - 1)

    with k.scope():
        X_ = NT
        trif = k.sb("trif2", [128, 128], F32)
        dma("sp", trif[:], I["tri"], W=[trif])
        trib = k.sb("trib2", [128, 128], BF16)
        op("dve", lambda e: e.tensor_copy(out=trib[:], in_=trif[:]), R=[trif], W=[trib])
        onesb = k.sb("onesb2", [128, 128], BF16)
        op("dve", lambda e: e.memset(onesb[:], 1.0), W=[onesb])
        eoff = k.sb("eoff2", [128, 32], F32)
        dma("sp", eoff[:], I["iota_e"], W=[eoff])
        op("dve", lambda e: e.tensor_scalar(out=eoff[:], in0=eoff[:], scalar1=float(CAP), scalar2=None, op0=OP.mult), R=[eoff], W=[eoff])
        pcolN = k.sb("pcolN2", [128, 1], F32)
        dma("sp", pcolN[:], I["idxcol"], W=[pcolN])
        op("dve", lambda e: e.tensor_scalar(out=pcolN[:], in0=pcolN[:], scalar1=float(NSLOT), scalar2=None, op0=OP.add), R=[pcolN], W=[pcolN])
        tokid = k.sb("tokid2", [128, NT], I32)
        dma("sp", tokid[:], I["tokid"], W=[tokid])
        tokrep = k.sb("tokrep2", [128, NT, 128], I32)
        op("dve", lambda e: e.tensor_copy(out=tokrep[:], in_=tokid[:].unsqueeze(2).to_broadcast([128, NT, 128])), R=[tokid], W=[tokrep])
        B_ = {}

        def T_(n, shp, dt=F32):
            B_[n] = k.sb("q_" + n, shp, dt)
            return B_[n]
        for n, shp in (("gmax", [128, X_]), ("gone", [128, X_, 4]), ("ge", [128, X_, 4]), ("gsum", [128, X_]), ("pg", [128, X_]),
                       ("ss", [128, X_, 8]), ("s2", [128, X_, 8]), ("v0", [128, X_]), ("m0", [128, X_, 8]), ("msk", [128, X_, 8]),
                       ("v1", [128, X_]), ("m1", [128, X_, 8]), ("e1", [128, X_]), ("den", [128, X_]), ("w", [128, X_, 2]),
                       ("M0", [128, X_, 32]), ("M1", [128, X_, 32]), ("M", [128, X_, 32]), ("Rk", [128, X_, 32]), ("t32", [128, X_, 32]),
                       ("base", [128, X_, 32]), ("csum", [128, X_, 32]),
                       ("rank", [128, X_, 2]), ("eo", [128, X_, 2]), ("valid", [128, X_, 2]), ("dg", [128, X_, 2]), ("ds", [128, X_, 2])):
            T_(n, shp)
        Mb = k.sb("q_Mb", [128, X_ * 32], BF16)
        dsi = k.sb("q_dsi", [128, X_, 2], I32)
        pq = [k.ps("pq%d" % i, [128, 512], F32) for i in range(4)]

        def dv(fn, Rb, Wb):
            op("dve", fn, R=[B_[n] if isinstance(n, str) else n for n in Rb], W=[B_[n] if isinstance(n, str) else n for n in Wb])
        G4 = L_all[:, :, 0:4]

        def bc(name, n):
            return B_[name][:].unsqueeze(2).to_broadcast([128, X_, n])
        dv(lambda e: e.tensor_reduce(out=B_["gmax"][:], in_=G4, axis=AX.X, op=OP.max), [L_all], ["gmax"])
        dv(lambda e: e.tensor_tensor(out=B_["gone"][:], in0=G4, in1=bc("gmax", 4), op=OP.is_equal), [L_all, "gmax"], ["gone"])
        dv(lambda e: e.tensor_tensor(out=B_["ge"][:], in0=G4, in1=bc("gmax", 4), op=OP.subtract), [L_all, "gmax"], ["ge"])
        op("act", lambda e: e.activation(out=B_["ge"][:], in_=B_["ge"][:], func=AF.Exp), R=[B_["ge"]], W=[B_["ge"]])
        dv(lambda e: e.tensor_reduce(out=B_["gsum"][:], in_=B_["ge"][:], axis=AX.X, op=OP.add), ["ge"], ["gsum"])
        dv(lambda e: e.reciprocal(out=B_["pg"][:], in_=B_["gsum"][:]), ["gsum"], ["pg"])
        for g in range(4):
            dst = "ss" if g == 0 else "s2"
            dv(lambda e, g=g, dst=dst: e.tensor_tensor(out=B_[dst][:], in0=L_all[:, :, 4 + 8 * g:12 + 8 * g],
                                                     in1=B_["gone"][:, :, g:g + 1].to_broadcast([128, X_, 8]), op=OP.mult),
               [L_all, "gone"], [dst])
            if g > 0:
                dv(lambda e: e.tensor_tensor(out=B_["ss"][:], in0=B_["ss"][:], in1=B_["s2"][:], op=OP.add), ["ss", "s2"], ["ss"])
        dv(lambda e: e.tensor_reduce(out=B_["v0"][:], in_=B_["ss"][:], axis=AX.X, op=OP.max), ["ss"], ["v0"])
        dv(lambda e: e.tensor_tensor(out=B_["m0"][:], in0=B_["ss"][:], in1=bc("v0", 8), op=OP.is_equal), ["ss", "v0"], ["m0"])
        dv(lambda e: e.scalar_tensor_tensor(out=B_["msk"][:].rearrange("p x e -> p (x e)"), in0=B_["m0"][:].rearrange("p x e -> p (x e)"),
                                            scalar=-1e30, in1=B_["ss"][:].rearrange("p x e -> p (x e)"), op0=OP.mult, op1=OP.add),
           ["m0", "ss"], ["msk"])
        dv(lambda e: e.tensor_reduce(out=B_["v1"][:], in_=B_["msk"][:], axis=AX.X, op=OP.max), ["msk"], ["v1"])
        dv(lambda e: e.tensor_tensor(out=B_["m1"][:], in0=B_["msk"][:], in1=bc("v1", 8), op=OP.is_equal), ["msk", "v1"], ["m1"])
        dv(lambda e: e.tensor_tensor(out=B_["e1"][:], in0=B_["v1"][:], in1=B_["v0"][:], op=OP.subtract), ["v1", "v0"], ["e1"])
        op("act", lambda e: e.activation(out=B_["e1"][:], in_=B_["e1"][:], func=AF.Exp), R=[B_["e1"]], W=[B_["e1"]])
        dv(lambda e: e.tensor_scalar(out=B_["den"][:], in0=B_["e1"][:], scalar1=1.0, scalar2=None, op0=OP.add), ["e1"], ["den"])
        dv(lambda e: e.reciprocal(out=B_["den"][:], in_=B_["den"][:]), ["den"], ["den"])
        dv(lambda e: e.tensor_tensor(out=B_["w"][:, :, 0], in0=B_["pg"][:], in1=B_["den"][:], op=OP.mult), ["pg", "den"], ["w"])
        dv(lambda e: e.tensor_tensor(out=B_["w"][:, :, 1], in0=B_["w"][:, :, 0], in1=B_["e1"][:], op=OP.mult), ["w", "e1"], ["w"])
        for (mn, Mn) in (("m0", "M0"), ("m1", "M1")):
            dv(lambda e, mn=mn, Mn=Mn: e.tensor_tensor(out=B_[Mn][:].rearrange("p x (g y) -> p x g y", y=8),
                                                      in0=B_["gone"][:].unsqueeze(3).to_broadcast([128, X_, 4, 8]),
                                                      in1=B_[mn][:].unsqueeze(2).to_broadcast([128, X_, 4, 8]), op=OP.mult),
               ["gone", mn], [Mn])
        dv(lambda e: e.tensor_tensor(out=B_["M"][:], in0=B_["M0"][:], in1=B_["M1"][:], op=OP.add), ["M0", "M1"], ["M"])
        dv(lambda e: e.tensor_copy(out=Mb[:], in_=B_["M"][:].rearrange("p x e -> p (x e)")), ["M"], [Mb])
        NHALF = (X_ * 32 + 511) // 512
        for hh in range(NHALF):
            c0, c1 = hh * 512, min((hh + 1) * 512, X_ * 32)
            op("pe", lambda e, hh=hh, c0=c0, c1=c1: e.matmul(pq[hh][:, 0:c1 - c0], lhsT=trib[:], rhs=Mb[:, c0:c1], start=True, stop=True),
               R=[trib, Mb], W=[pq[hh]])
            op("pe", lambda e, hh=hh, c0=c0, c1=c1: e.matmul(pq[2 + hh][:, 0:c1 - c0], lhsT=onesb[:], rhs=Mb[:, c0:c1], start=True, stop=True),
               R=[onesb, Mb], W=[pq[2 + hh]])
            dv(lambda e, hh=hh, c0=c0, c1=c1: e.tensor_copy(out=B_["Rk"][:].rearrange("p x e -> p (x e)")[:, c0:c1], in_=pq[hh][:, 0:c1 - c0]),
               [pq[hh]], ["Rk"])
            dv(lambda e, hh=hh, c0=c0, c1=c1: e.tensor_copy(out=B_["csum"][:].rearrange("p x e -> p (x e)")[:, c0:c1], in_=pq[2 + hh][:, 0:c1 - c0]),
               [pq[2 + hh]], ["csum"])
        dv(lambda e: e.memset(B_["base"][:, 0, :], 0.0), [], ["base"])
        for i in range(1, X_):
            dv(lambda e, i=i: e.tensor_tensor(out=B_["base"][:, i, :], in0=B_["base"][:, i - 1, :], in1=B_["csum"][:, i - 1, :], op=OP.add),
               ["base", "csum"], ["base"])
        dv(lambda e: e.tensor_tensor(out=B_["Rk"][:], in0=B_["Rk"][:], in1=B_["base"][:], op=OP.add), ["Rk", "base"], ["Rk"])
        for a_, Mn in enumerate(("M0", "M1")):
            dv(lambda e, Mn=Mn: e.tensor_tensor(out=B_["t32"][:], in0=B_[Mn][:], in1=B_["Rk"][:], op=OP.mult), [Mn, "Rk"], ["t32"])
            dv(lambda e, a_=a_: e.tensor_reduce(out=B_["rank"][:, :, a_], in_=B_["t32"][:], axis=AX.X, op=OP.add), ["t32"], ["rank"])
            dv(lambda e, Mn=Mn: e.tensor_tensor(out=B_["t32"][:], in0=B_[Mn][:], in1=eoff[:].unsqueeze(1).to_broadcast([128, X_, 32]),
                                                op=OP.mult), [Mn, eoff], ["t32"])
            dv(lambda e, a_=a_: e.tensor_reduce(out=B_["eo"][:, :, a_], in_=B_["t32"][:], axis=AX.X, op=OP.add), ["t32"], ["eo"])
        dv(lambda e: e.tensor_scalar(out=B_["valid"][:], in0=B_["rank"][:], scalar1=float(CAP), scalar2=None, op0=OP.is_lt), ["rank"], ["valid"])
        dv(lambda e: e.tensor_tensor(out=B_["dg"][:], in0=B_["rank"][:], in1=B_["eo"][:], op=OP.add), ["rank", "eo"], ["dg"])
        dv(lambda e: e.tensor_tensor(out=B_["dg"][:], in0=B_["dg"][:], in1=B_["valid"][:], op=OP.mult), ["dg", "valid"], ["dg"])
        dv(lambda e: e.tensor_tensor(out=WT[:], in0=B_["w"][:], in1=B_["valid"][:], op=OP.mult), ["w", "valid"], [WT])
        dv(lambda e: e.tensor_copy(out=DEST[:], in_=B_["dg"][:]), ["dg"], [DEST])
        dv(lambda e: e.tensor_scalar(out=B_["ds"][:], in0=B_["valid"][:], scalar1=-1.0, scalar2=1.0, op0=OP.mult, op1=OP.add), ["valid"], ["ds"])
        dv(lambda e: e.scalar_tensor_tensor(out=B_["ds"][:].rearrange("p x a -> p (x a)"), in0=B_["ds"][:].rearrange("p x a -> p (x a)"),
                                            scalar=pcolN[:, 0:1], in1=B_["dg"][:].rearrange("p x a -> p (x a)"), op0=OP.mult, op1=OP.add),
           ["ds", "dg", pcolN], ["ds"])
        dv(lambda e: e.tensor_copy(out=dsi[:], in_=B_["ds"][:]), ["ds"], [dsi])
        for i in range(X_):
            for a_ in range(2):
                dma("pool", S["slot"][:, :], tokrep[:, i, :], R=[tokrep, dsi],
                    indirect=dict(out_offset=bass.IndirectOffsetOnAxis(ap=dsi[:, i, a_:a_ + 1], axis=0), in_offset=None))
        if "rt" in dbg:
            dbt = k.sb("dbt", [128, X_, 40], F32)
            dv(lambda e: e.tensor_copy(out=dbt[:, :, 0:36], in_=L_all[:]), [L_all], [dbt])
            dv(lambda e: e.tensor_copy(out=dbt[:, :, 36:38], in_=B_["w"][:]), ["w"], [dbt])
            dv(lambda e: e.tensor_copy(out=dbt[:, :, 38:40], in_=B_["dg"][:]), ["dg"], [dbt])
            dma("sp", S["rt"].rearrange("(x p) c -> p x c", p=128), dbt[:], R=[dbt])
    if stop_after == "C":
        k.close()
        return nc

    CAPB = cfg.CAPB
    with k.scope():
        W1 = [k.sb("W1_%d" % i, [128, KC, FF], BF16) for i in range(2)]
        W3 = [k.sb("W3_%d" % i, [128, KC, FF], BF16) for i in range(2)]
        W2 = [k.sb("W2_%d" % i, [128, FC, D], BF16) for i in range(2)]
        idx = [k.sb("idx%d" % i, [128, CAPB], I32) for i in range(2)]
        xb = [k.sb("xb%d" % i, [128, D], BF16) for i in range(4)]
        xT = k.sb("xT", [128, KC, CAP], BF16)
        sg = [k.sb("sg%d" % i, [128, CAP], F32) for i in range(2)]
        actT = k.sb("actT", [128, FC, CAP], BF16)
        ysb = [k.sb("ysb%d" % i, [128, D], BF16) for i in range(2)]
        ptx = [k.ps("ptx%d" % i, [128, 1024], BF16) for i in range(2)]
        pu1 = k.ps("pu1", [128, 512], F32)
        pu3 = k.ps("pu3", [128, 512], F32)
        pyy = [k.ps("pyy%d" % i, [128, 512], F32) for i in range(4)]

        opg2e = k.sb("opg2e", [128, D], F32)
        bc_load("sp", opg2e, S["vec"][VEC["opg2"]:VEC["opg2"] + 1, :])

        def load_expert(e_):
            b = e_ % 2
            w1v = I["w1"][e_].rearrange("(kc p) f -> p kc f", p=128)
            w3v = I["w3"][e_].rearrange("(kc p) f -> p kc f", p=128)
            w2v = I["w2"][e_].rearrange("(fc p) n -> p fc n", p=128)
            dma("pool", W1[b][:], w1v, W=[W1[b]])
            dma("pool", W3[b][:], w3v, W=[W3[b]])
            for j in range(4):
                dma("pool", W2[b][:, :, j * 512:(j + 1) * 512], w2v[:, :, j * 512:(j + 1) * 512], W=[W2[b]])

        load_expert(0)
        ycount = 0
        for e_ in range(NE):
            b = e_ % 2
            for blk in range(CAPB):
                r0 = e_ * CAP + blk * 128
                dma("sp", idx[b][:, blk:blk + 1], S["slot"][r0:r0 + 128, 0:1], W=[idx[b]], allow_slow_non_contiguous=True)
            for blk in range(CAPB):
                XB = xb[blk % 4]
                dma("pool", XB[:], S["h2"][:, :], R=[idx[b]], W=[XB],
                    indirect=dict(out_offset=None, in_offset=bass.IndirectOffsetOnAxis(ap=idx[b][:, blk:blk + 1], axis=0)))
                for g in range(2):
                    p = ptx[g]
                    for j in range(8):
                        c = g * 8 + j
                        op("pe", lambda e, c=c, j=j, p=p, XB=XB: e.transpose(out=p[:, j * 128:(j + 1) * 128], in_=XB[:, c * 128:(c + 1) * 128],
                                                                            identity=ident_b[:]), R=[XB, ident_b], W=[p])
                    dstv = xT[:, g * 8:(g + 1) * 8, blk * 128:(blk + 1) * 128]
                    srcv = p[:].rearrange("p (c t) -> p c t", t=128)
                    if g == 0:
                        op("act", lambda e, srcv=srcv, dstv=dstv: e.copy(out=dstv, in_=srcv), R=[p], W=[xT])
                    else:
                        op("dve", lambda e, srcv=srcv, dstv=dstv: e.tensor_copy(out=dstv, in_=srcv), R=[p], W=[xT])
            if e_ + 1 < NE:
                load_expert(e_ + 1)
            for m in range(FC):
                for (Wm, pu) in ((W1[b], pu1), (W3[b], pu3)):
                    for kc in range(KC):
                        op("pe", lambda e, kc=kc, m=m, Wm=Wm, pu=pu: e.matmul(pu[:, 0:CAP], lhsT=Wm[:, kc, m * 128:(m + 1) * 128], rhs=xT[:, kc, :],
                                                                              start=(kc == 0), stop=(kc == KC - 1)), R=[Wm, xT], W=[pu])
                SG = sg[m % 2]
                op("act", lambda e, SG=SG: e.activation(out=SG[:], in_=pu1[:, 0:CAP], func=AF.Silu), R=[pu1], W=[SG])
                op("dve", lambda e, m=m, SG=SG: e.tensor_tensor(out=actT[:, m, :], in0=pu3[:, 0:CAP], in1=SG[:], op=OP.mult), R=[pu3, SG], W=[actT])
            for blk in range(CAPB):
                Y = ysb[ycount % 2]
                ycount += 1
                for n in range(4):
                    p = pyy[n]
                    for m in range(FC):
                        op("pe", lambda e, n=n, m=m, p=p, blk=blk: e.matmul(p[:], lhsT=actT[:, m, blk * 128:(blk + 1) * 128],
                                                                           rhs=W2[b][:, m, n * 512:(n + 1) * 512], start=(m == 0),
                                                                           stop=(m == FC - 1)), R=[actT, W2[b]], W=[p])
                    op("dve", lambda e, n=n, p=p, Y=Y: e.tensor_tensor(out=Y[:, n * 512:(n + 1) * 512], in0=p[:],
                                                                       in1=opg2e[:, n * 512:(n + 1) * 512], op=OP.mult), R=[p, opg2e], W=[Y])
                r0 = e_ * CAP + blk * 128
                dma("act", S["y"][r0:r0 + 128, :], Y[:], R=[Y])
    if stop_after == "E":
        k.close()
        return nc

    with k.scope():
        rows = {}
        for n in ("ln2_g", "ln2_b"):
            rows[n] = k.sb("rowf_" + n, [128, D], F32)
            bc_load("sp", rows[n], S["vec"][VEC[n]:VEC[n] + 1, :])
        g0 = [k.sb("g0_%d" % i, [128, D], BF16) for i in range(2)]
        g1 = [k.sb("g1_%d" % i, [128, D], BF16) for i in range(2)]
        gf = [k.sb("gf_%d" % i, [128, D], F32) for i in range(2)]
        x1t = [k.sb("x1t%d" % i, [128, D], F32) for i in range(2)]
        ot = [k.sb("ot%d" % i, [128, D], F32) for i in range(2)]
        st = k.sb("stf", [128, 4, 6], F32)
        mv = k.sb("mvf", [128, 2], F32)
        rstd = k.sb("rstdf", [128, 1], F32)
        nmr = k.sb("nmrf", [128, 1], F32)
        def issue_loads(i):
            for a_, G in enumerate((g0[i % 2], g1[i % 2])):
                dma("pool", G[:], S["y"][:, :], R=[DEST], W=[G],
                    indirect=dict(out_offset=None, in_offset=bass.IndirectOffsetOnAxis(ap=DEST[:, i, a_:a_ + 1], axis=0)))
            dma("sp", x1t[i % 2][:], S["x1"][i * 128:(i + 1) * 128, :], W=[x1t[i % 2]])

        issue_loads(0)
        for i in range(NT):
            rws = slice(i * 128, (i + 1) * 128)
            G0, G1, X1, OT = g0[i % 2], g1[i % 2], x1t[i % 2], ot[i % 2]
            GF = gf[i % 2]
            op("act", lambda e, G0=G0, GF=GF: e.activation(out=GF[:], in_=G0[:], func=AF.Identity, scale=WT[:, i, 0:1]), R=[G0, WT], W=[GF])
            op("dve", lambda e, GF=GF, G1=G1: e.scalar_tensor_tensor(out=GF[:], in0=G1[:], scalar=WT[:, i, 1:2], in1=GF[:], op0=OP.mult,
                                                                    op1=OP.add), R=[GF, G1, WT], W=[GF])
            op("dve", lambda e, GF=GF, X1=X1: e.scalar_tensor_tensor(out=X1[:], in0=X1[:], scalar=ALPHA, in1=GF[:], op0=OP.mult, op1=OP.add),
               R=[X1, GF], W=[X1])
            if i + 1 < NT:
                issue_loads(i + 1)
            for c4 in range(4):
                op("dve", lambda e, c4=c4, X1=X1: e.bn_stats(out=st[:, c4, :], in_=X1[:, c4 * 512:(c4 + 1) * 512]), R=[X1], W=[st])
            op("dve", lambda e: e.bn_aggr(out=mv[:], in_=st[:].rearrange("p a b -> p (a b)")), R=[st], W=[mv])
            op("act", lambda e: e.activation(out=rstd[:], in_=mv[:, 1:2], func=AF.Sqrt, bias=LN_EPS_AP[:], scale=1.0), R=[mv, LN_EPS_B], W=[rstd])
            op("dve", lambda e: e.reciprocal(out=rstd[:], in_=rstd[:]), R=[rstd], W=[rstd])
            op("dve", lambda e: e.tensor_scalar(out=nmr[:], in0=mv[:, 0:1], scalar1=rstd[:], scalar2=-1.0, op0=OP.mult, op1=OP.mult),
               R=[mv, rstd], W=[nmr])
            op("act", lambda e, X1=X1, OT=OT: e.activation(out=OT[:], in_=X1[:], func=AF.Identity, bias=nmr[:], scale=rstd[:]),
               R=[X1, nmr, rstd], W=[OT])
            op("dve", lambda e, OT=OT: e.tensor_tensor(out=OT[:], in0=OT[:], in1=rows["ln2_g"][:], op=OP.mult), R=[OT, rows["ln2_g"]], W=[OT])
            op("pool", lambda e, OT=OT: e.tensor_tensor(out=OT[:], in0=OT[:], in1=rows["ln2_b"][:], op=OP.add), R=[OT, rows["ln2_b"]], W=[OT])
            dma("sp", out_d[rws, :], OT[:], R=[OT])

    k.close()
    return nc


def kernel(**inputs):
    cfg = Cfg()
    maps = prep_inputs(inputs, cfg)
    nc = build(cfg)
    res = run_bass_kernel_spmd(nc, maps, core_ids=list(range(8)))
    out = np.zeros((cfg.B, 2 * cfg.SL, D), np.float32)
    for b in range(cfg.B):
        for hf in range(2):
            out[b, hf * cfg.SL:(hf + 1) * cfg.SL] = res.results[b * 2 + hf]["out"]
    return out
```

```python
import contextlib
import os
KSKIP = set(os.environ.get('KSKIP', '').split(','))
KTILES = int(os.environ.get('KTILES', '0'))
import math
import numpy as np
import ml_dtypes
import concourse.bass as bass
import concourse.mybir as mybir
from concourse.bass_utils import run_bass_kernel_spmd

F32 = mybir.dt.float32
BF16 = mybir.dt.bfloat16
I32 = mybir.dt.int32
AF = mybir.ActivationFunctionType
OP = mybir.AluOpType
AX = mybir.AxisListType

D = 2048
KC = 16
NHALO = 8
LN_EPS = 1e-5
ALPHA = 2.0 ** 0.25
NEG = -30000.0


class Cfg:
    def __init__(self, NT=32, FF=512, CAPB=4, B=4):
        self.NT = NT
        self.NO = NT
        self.FF = FF
        self.CAPB = CAPB
        self.B = B
        self.SL = NT * 128
        self.SA = self.SL + 2048
        self.NE = 32
        self.CAP = CAPB * 128


class Tick:
    __slots__ = ("key", "sem", "val")

    def __init__(self, key, sem, val):
        self.key = key
        self.sem = sem
        self.val = val


class Buf:
    def __init__(self, h, name=""):
        self.h = h
        self.name = name
        self.w = None
        self.r = {}
        self.psum = False

    def __getitem__(self, idx):
        return self.h[idx]


class Eng:
    def __init__(self, key, eng, sem, is_pe=False):
        self.key = key
        self.eng = eng
        self.sem = sem
        self.cnt = 0
        self.seen = {}
        self.is_pe = is_pe
        self.dsems = []
        self.dvals = []
        self.dma_i = 0


NDS = 20


class K:
    def __init__(self, nc):
        self.nc = nc
        self.es = contextlib.ExitStack()
        self.E = {}
        for key, eng, pe in (("pe", nc.tensor, True), ("act", nc.scalar, False), ("dve", nc.vector, False),
                             ("pool", nc.gpsimd, False), ("sp", nc.sync, False)):
            sem = self.es.enter_context(nc.semaphore("s_" + key))
            self.E[key] = Eng(key, eng, sem, pe)
        for q in ("sp", "act", "pool"):
            e = self.E[q]
            for i in range(NDS):
                e.dsems.append(self.es.enter_context(nc.semaphore("d_%s%d" % (q, i))))
                e.dvals.append(0)
        self.scopes = []
        self.uid = 0

    def _stack(self):
        return self.scopes[-1] if self.scopes else self.es

    @contextlib.contextmanager
    def scope(self):
        st = contextlib.ExitStack()
        self.scopes.append(st)
        try:
            yield
            self.fence()
        finally:
            self.scopes.pop()
            st.close()

    def sb(self, name, shape, dt):
        self.uid += 1
        h = self._stack().enter_context(self.nc.sbuf_tensor("%s_%d" % (name, self.uid), list(shape), dt))
        return Buf(h, name)

    def ps(self, name, shape, dt):
        self.uid += 1
        h = self._stack().enter_context(self.nc.psum_tensor("%s_%d" % (name, self.uid), list(shape), dt))
        b = Buf(h, name)
        b.psum = True
        return b

    def _wait(self, E, ticks):
        need = {}
        for t in ticks:
            if t is None:
                continue
            if E.is_pe and t.key == E.key:
                continue
            cur = need.get(t.key)
            if cur is None or cur[1] < t.val:
                need[t.key] = (t.sem, t.val)
        for key, (sem, val) in need.items():
            if E.seen.get(key, 0) >= val:
                continue
            E.eng.wait_ge(sem, val)
            E.seen[key] = val

    def _deps(self, E, R, W):
        deps = []
        for b in R:
            deps.append(b.w)
            if b.psum:
                for k, t in b.r.items():
                    if k != E.key:
                        deps.append(t)
        for b in W:
            deps.append(b.w)
            for k, t in b.r.items():
                deps.append(t)
        return deps

    def _commit(self, E, t, R, W):
        for b in R:
            if b not in W:
                b.r[E.key if not isinstance(t.key, tuple) else t.key] = t
        for b in W:
            b.w = t
            b.r = {}

    def op(self, en, fn, R=(), W=()):
        E = self.E[en]
        self._wait(E, self._deps(E, R, W))
        ins = fn(E.eng)
        E.cnt += 1
        ins.then_inc(E.sem, 1)
        t = Tick(E.key, E.sem, E.cnt)
        self._commit(E, t, R, W)
        return t

    def dma(self, q, out, in_, R=(), W=(), indirect=None, **kw):
        E = self.E[q]
        self._wait(E, self._deps(E, R, W))
        slot = E.dma_i % NDS
        E.dma_i += 1
        sem = E.dsems[slot]
        prev = E.dvals[slot]
        key = ("d", q, slot)
        if prev > 0 and E.seen.get(key, 0) < prev:
            E.eng.wait_ge(sem, prev)
            E.seen[key] = prev
        if indirect is None:
            ins = E.eng.dma_start(out=out, in_=in_, **kw)
        else:
            ins = E.eng.indirect_dma_start(out=out, in_=in_, **indirect)
        ins.then_inc(sem, 16)
        E.dvals[slot] = prev + 16
        t = Tick(key, sem, prev + 16)
        self._commit(E, t, R, W)
        return t

    def fence(self):
        ticks = []
        for key, e in self.E.items():
            if e.cnt > 0:
                ticks.append(Tick(e.key, e.sem, e.cnt))
            for i, v in enumerate(e.dvals):
                if v > 0:
                    ticks.append(Tick(("d", key, i), e.dsems[i], v))
        for key, e in self.E.items():
            self._wait(e, [t for t in ticks if not (t.key == e.key)])

    def close(self):
        self.fence()
        self.es.close()


def _bf(a):
    return np.asarray(a, dtype=np.float32).astype(ml_dtypes.bfloat16)


def host_consts(cfg, hf):
    c = {}
    a = np.arange(128)[:, None]
    i = np.arange(128)[None, :]
    mA = (a >= i)
    mB = (a <= i)
    maskL = 1.0 if hf == 1 else 0.0
    maskR = 1.0 if hf == 0 else 0.0
    mAe = mA & ((a >= 64) | (maskL > 0))
    mBe = mB & ((a < 64) | (maskR > 0))
    neg = np.stack([np.where(m, 0.0, NEG) for m in (mA, mB, mAe, mBe)], axis=1)
    c["negmask"] = _bf(neg)
    c["ident_f"] = np.eye(128, dtype=np.float32)
    c["ident_b"] = _bf(np.eye(128))
    inv_rope = (500000.0 ** (-np.arange(0, 32, 2, dtype=np.float32) / 32.0)).astype(np.float32)
    inv_ret = (10000.0 ** (-np.linspace(0.0, 1.0, 128, dtype=np.float32))).astype(np.float32)
    invf = np.concatenate([inv_ret, inv_rope]).astype(np.float32) / np.float32(2.0 * math.pi)
    c["invf"] = np.broadcast_to(invf[None, :], (128, 144)).astype(np.float32).copy()
    idx = np.arange(128, dtype=np.float32)
    dm = idx[None, :] - idx[:, None]
    c["dpos"] = np.maximum(dm, 0.0).astype(np.float32)
    c["dneg"] = np.maximum(-dm, 0.0).astype(np.float32)
    c["mfw"] = (dm >= 0).astype(np.float32)
    c["mbw"] = (dm < 0).astype(np.float32)
    c["idxcol"] = idx[:, None].astype(np.float32).copy()
    c["idxrow"] = np.broadcast_to(idx[None, :], (128, 128)).astype(np.float32).copy()
    c["flags"] = np.array([[1.0 if hf == 1 else 0.0, 1.0 if hf == 0 else 0.0]], np.float32).repeat(128, 0)
    c["tri"] = (np.arange(128)[:, None] < np.arange(128)[None, :]).astype(np.float32)
    c["ones_f"] = np.ones((128, 128), np.float32)
    c["iota_e"] = np.broadcast_to(np.arange(32, dtype=np.float32)[None, :], (128, 32)).copy()
    c["tokid"] = (np.arange(cfg.NT)[None, :] * 128 + np.arange(128)[:, None]).astype(np.int32)
    return c


def prep_inputs(inp, cfg):
    x = np.asarray(inp["x"])
    B, S, _ = x.shape
    SL = cfg.SL
    assert S == 2 * SL
    pos = np.asarray(inp["positions"])
    maps = []
    f32 = lambda a: np.ascontiguousarray(np.asarray(a, dtype=np.float32))
    shared = {
        "w_ada": f32(inp["w_ada"][0]), "b_ada": f32(inp["b_ada"][0])[None, :],
        "w_in": f32(inp["w_in"][0]), "w_out": f32(inp["w_out"][0]),
        "ln0_g": f32(inp["ln0_g"])[None, :], "ln0_b": f32(inp["ln0_b"])[None, :],
        "ln1_g": f32(inp["ln1_g"][0])[None, :], "ln1_b": f32(inp["ln1_b"][0])[None, :],
        "ln2_g": f32(inp["ln2_g"][0])[None, :], "ln2_b": f32(inp["ln2_b"][0])[None, :],
        "w1": f32(inp["w1"][0]), "w3": f32(inp["w3"][0]), "w2": f32(inp["w2"][0]),
        "lg_f": f32(inp["ret_log_decay_f"][0])[None, :], "lg_b": f32(inp["ret_log_decay_b"][0])[None, :],
    }
    wr = np.concatenate([f32(inp["w_group"][0]),
                         np.transpose(f32(inp["w_sub"][0]), (1, 0, 2)).reshape(D, 32)], axis=1)
    br = np.concatenate([f32(inp["b_group"][0]), f32(inp["b_sub"][0]).reshape(32)])[None, :]
    shared["w_r"] = np.ascontiguousarray(wr)
    shared["b_r"] = np.ascontiguousarray(br)
    for b in range(B):
        for hf in range(2):
            T0 = hf * SL
            m = dict(shared)
            m["x_own"] = np.ascontiguousarray(x[b, T0:T0 + SL])
            if hf == 0:
                oth = np.arange(SL, 2 * SL)
                dist = (oth - SL).astype(np.float32)
                m["lg_oth"] = shared["lg_b"]
            else:
                oth = np.concatenate([np.arange(SL - 1024, SL), np.arange(0, SL - 1024)])
                dist = (SL - 1 - oth).astype(np.float32)
                m["lg_oth"] = shared["lg_f"]
            m["x_oth"] = np.ascontiguousarray(x[b, oth])
            m["pos_own"] = np.ascontiguousarray(pos[b, T0:T0 + SL].reshape(cfg.NT, 128).T.astype(np.int32))
            m["pos_oth"] = np.ascontiguousarray(pos[b, oth].reshape(cfg.NO, 128).T.astype(np.int32))
            m["dist_oth"] = np.ascontiguousarray(dist.reshape(cfg.NO, 128).T)
            m["c_col"] = np.ascontiguousarray(np.asarray(inp["c"], np.float32)[b].reshape(KC, 128).T)
            m.update(host_consts(cfg, hf))
            maps.append(m)
    return maps


def build(cfg, dbg=None, stop_after=None):
    dbg = dbg or []
    nc = bass.Bass("TRN2", target_bir_lowering=False)
    NT, NO, SL, SA, FF, NE, CAP = cfg.NT, cfg.NO, cfg.SL, cfg.SA, cfg.FF, cfg.NE, cfg.CAP
    FC = FF // 128

    def din(name, shape, dt=F32):
        return nc.dram_tensor(name, list(shape), dt, kind="ExternalInput").ap()

    def dscr(name, shape, dt):
        kind = "ExternalOutput" if name in dbg else "Internal"
        return nc.dram_tensor(name, list(shape), dt, kind=kind).ap()

    I = {}
    I["x_own"] = din("x_own", [SL, D])
    I["x_oth"] = din("x_oth", [NO * 128, D])
    I["pos_own"] = din("pos_own", [128, NT], I32)
    I["pos_oth"] = din("pos_oth", [128, NO], I32)
    I["dist_oth"] = din("dist_oth", [128, NO])
    I["lg_oth"] = din("lg_oth", [1, 4])
    I["lg_f"] = din("lg_f", [1, 4])
    I["lg_b"] = din("lg_b", [1, 4])
    I["c_col"] = din("c_col", [128, KC])
    I["w_ada"] = din("w_ada", [D, 6 * D])
    I["b_ada"] = din("b_ada", [1, 6 * D])
    I["w_in"] = din("w_in", [D, 7168])
    I["w_out"] = din("w_out", [D, D])
    for n in ("ln0_g", "ln0_b", "ln1_g", "ln1_b", "ln2_g", "ln2_b"):
        I[n] = din(n, [1, D])
    I["w1"] = din("w1", [NE, D, FF])
    I["w3"] = din("w3", [NE, D, FF])
    I["w2"] = din("w2", [NE, FF, D])
    I["w_r"] = din("w_r", [D, 36])
    I["b_r"] = din("b_r", [1, 36])
    I["negmask"] = din("negmask", [128, 4, 128], BF16)
    I["ident_f"] = din("ident_f", [128, 128])
    I["ident_b"] = din("ident_b", [128, 128], BF16)
    I["invf"] = din("invf", [128, 144])
    for n in ("dpos", "dneg", "mfw", "mbw", "idxrow", "tri", "ones_f"):
        I[n] = din(n, [128, 128])
    I["idxcol"] = din("idxcol", [128, 1])
    I["flags"] = din("flags", [128, 2])
    I["iota_e"] = din("iota_e", [128, 32])
    I["tokid"] = din("tokid", [128, NT], I32)

    out_d = nc.dram_tensor("out", [SL, D], F32, kind="ExternalOutput").ap()

    S = {}
    S["vec"] = dscr("vec", [16, D], F32)
    S["ak"] = dscr("ak", [SA, 1024], BF16)
    S["av"] = dscr("av", [SA, 1024], BF16)
    S["rk"] = dscr("rk", [SL, 1024], BF16)
    S["rv"] = dscr("rv", [SL, 1024], BF16)
    S["aq"] = dscr("aq", [SL, 1024], BF16)
    S["rq"] = dscr("rq", [SL, 1024], BF16)
    S["rg"] = dscr("rg", [SL, 1024], BF16)
    S["xn"] = dscr("xn", [SL, D], F32)
    S["sin"] = dscr("sin", [4, 2, 128, 256], F32)

    k = K(nc)
    op, dma = k.op, k.dma

    ident_f = k.sb("ident_f", [128, 128], F32)
    ident_b = k.sb("ident_b", [128, 128], BF16)
    dma("sp", ident_f[:], I["ident_f"], W=[ident_f])
    dma("sp", ident_b[:], I["ident_b"], W=[ident_b])
    cols = k.sb("cols", [128, 4, KC], F32)

    VEC = {n: i for i, n in enumerate(["ln0_g", "ln0_b", "opg1", "G2", "B2", "ln1_g", "ln1_b", "opg2",
                                       "ln2_g", "ln2_b"])}

    def bc_load(q, dst, src_row):
        return dma(q, dst[:], src_row.partition_broadcast(128), W=[dst])

    NSLOT = NE * CAP
    S["slot"] = dscr("slot", [NSLOT + 128, 128], I32)
    zi = k.sb("zi", [128, 128], I32)
    op("pool", lambda e: e.memset(zi[:], 0), W=[zi])

    with k.scope():
        cc = k.sb("cc", [128, KC], F32)
        dma("sp", cc[:], I["c_col"], W=[cc])
        cs = k.sb("cs", [128, KC], F32)
        op("act", lambda e: e.activation(out=cs[:], in_=cc[:], func=AF.Silu), R=[cc], W=[cs])
        for b in range(NSLOT // 128):
            dma("act", S["slot"][b * 128:(b + 1) * 128, :], zi[:], R=[zi])
        csb = k.sb("csb", [128, KC, 128], BF16)
        op("dve", lambda e: e.tensor_copy(out=csb[:], in_=cs[:].unsqueeze(2).to_broadcast([128, KC, 128])),
           R=[cs], W=[csb])
        mod = k.sb("mod", [128, 6 * D], F32)
        bsl = [k.sb("bsl%d" % i, [128, 512], F32) for i in range(2)]
        wa = [k.sb("wa%d" % i, [128, KC, 512], BF16) for i in range(2)]
        pm = [k.ps("pm%d" % i, [128, 512], F32) for i in range(2)]
        wada_v = I["w_ada"].rearrange("(kc p) n -> p kc n", p=128)
        for s in range(24):
            w = wa[s % 2]
            bs = bsl[s % 2]
            dma("pool", w[:], wada_v[:, :, s * 512:(s + 1) * 512], W=[w])
            dma("sp", bs[:], I["b_ada"][0:1, s * 512:(s + 1) * 512].partition_broadcast(128), W=[bs])
            p = pm[s % 2]
            for kc in range(KC):
                op("pe", lambda e, kc=kc, p=p, w=w: e.matmul(p[:], lhsT=csb[:, kc, :], rhs=w[:, kc, :],
                                                             start=(kc == 0), stop=(kc == KC - 1)),
                   R=[csb, w], W=[p])
            op("dve", lambda e, p=p, s=s, bs=bs: e.tensor_tensor(out=mod[:, s * 512:(s + 1) * 512], in0=p[:], in1=bs[:],
                                                                 op=OP.add), R=[p, bs], W=[mod])
        sh1, sc1, g1, sh2, sc2, g2 = [mod[:, i * D:(i + 1) * D] for i in range(6)]
        lnv = {}
        for n in ("ln0_g", "ln0_b", "ln1_g", "ln1_b"):
            lnv[n] = k.sb(n, [128, D], F32)
            bc_load("sp", lnv[n], I[n])
        t1 = k.sb("t1", [128, D], F32)
        t2 = k.sb("t2", [128, D], F32)
        t3 = k.sb("t3", [128, D], F32)

        def diag_extract(src_ap, srcbufs, ci):
            op("dve", lambda e: e.tensor_tensor(out=t3[:].rearrange("p (c q) -> p c q", q=128),
                                                in0=src_ap.rearrange("p (c q) -> p c q", q=128),
                                                in1=ident_f[:].unsqueeze(1).to_broadcast([128, KC, 128]),
                                                op=OP.mult), R=srcbufs + [ident_f], W=[t3])
            op("dve", lambda e: e.tensor_reduce(out=cols[:, ci, :], in_=t3[:].rearrange("p (c q) -> p c q", q=128),
                                                axis=AX.X, op=OP.add), R=[t3], W=[cols])

        def store_vec(name, src_ap, bufs):
            dma("sp", S["vec"][VEC[name]:VEC[name] + 1, :], src_ap[0:1, :], R=bufs)

        op("dve", lambda e: e.tensor_scalar(out=t1[:], in0=sc1, scalar1=1.0, scalar2=None, op0=OP.add), R=[mod], W=[t1])
        op("dve", lambda e: e.tensor_tensor(out=t2[:], in0=t1[:], in1=lnv["ln0_g"][:], op=OP.mult), R=[t1, lnv["ln0_g"]], W=[t2])
        diag_extract(t2[:], [t2], 0)
        op("dve", lambda e: e.tensor_tensor(out=t2[:], in0=t1[:], in1=lnv["ln0_b"][:], op=OP.mult), R=[t1, lnv["ln0_b"]], W=[t2])
        op("dve", lambda e: e.tensor_tensor(out=t2[:], in0=t2[:], in1=sh1, op=OP.add), R=[t2, mod], W=[t2])
        diag_extract(t2[:], [t2], 1)
        op("dve", lambda e: e.tensor_scalar(out=t1[:], in0=sc2, scalar1=1.0, scalar2=None, op0=OP.add), R=[mod], W=[t1])
        op("dve", lambda e: e.tensor_tensor(out=t2[:], in0=t1[:], in1=lnv["ln1_g"][:], op=OP.mult), R=[t1, lnv["ln1_g"]], W=[t2])
        diag_extract(t2[:], [t2], 2)
        store_vec("G2", t2, [t2])
        t4 = k.sb("t4", [128, D], F32)
        op("dve", lambda e: e.tensor_tensor(out=t4[:], in0=t1[:], in1=lnv["ln1_b"][:], op=OP.mult), R=[t1, lnv["ln1_b"]], W=[t4])
        op("dve", lambda e: e.tensor_tensor(out=t4[:], in0=t4[:], in1=sh2, op=OP.add), R=[t4, mod], W=[t4])
        diag_extract(t4[:], [t4], 3)
        store_vec("B2", t4, [t4])
        t5 = t2
        op("dve", lambda e: e.tensor_scalar(out=t5[:], in0=g1, scalar1=1.0, scalar2=None, op0=OP.add), R=[mod], W=[t5])
        store_vec("opg1", t5, [t5])
        t6 = t4
        op("dve", lambda e: e.tensor_scalar(out=t6[:], in0=g2, scalar1=1.0, scalar2=None, op0=OP.add), R=[mod], W=[t6])
        store_vec("opg2", t6, [t6])
        for n in ("ln0_g", "ln0_b", "ln1_g", "ln1_b", "ln2_g", "ln2_b"):
            dma("sp", S["vec"][VEC[n]:VEC[n] + 1, :], I[n])
    if stop_after == "pre":
        k.close()
        return nc

    win_v = I["w_in"].rearrange("(kc p) n -> p kc n", p=128)

    def load_w(dst, col0, dcol0, ncols):
        for j in range(ncols // 512):
            dma("pool", dst[:, :, dcol0 + j * 512: dcol0 + (j + 1) * 512],
                win_v[:, :, col0 + j * 512: col0 + (j + 1) * 512], W=[dst])

    def phaseA(pass_id):
        with k.scope():
            if pass_id == 1:
                NCOL = 4096
                W = k.sb("WA", [128, KC, NCOL], BF16)
                load_w(W, 1024, 0, 2048)
                load_w(W, 4096, 2048, 2048)
                tiles = [("oth", j) for j in range(NO)] + [("own", i) for i in range(NT)]
            else:
                NCOL = 3072
                W = k.sb("WB", [128, KC, NCOL], BF16)
                load_w(W, 0, 0, 1024)
                load_w(W, 3072, 1024, 1024)
                load_w(W, 6144, 2048, 1024)
                tiles = [("own", i) for i in range(NT)]
                g0 = k.sb("g0", [128, D], F32)
                b0 = k.sb("b0", [128, D], F32)
                bc_load("sp", g0, S["vec"][VEC["ln0_g"]:VEC["ln0_g"] + 1, :])
                bc_load("sp", b0, S["vec"][VEC["ln0_b"]:VEC["ln0_b"] + 1, :])
            if KTILES:
                tiles = [t for t in tiles if t[1] < KTILES or (t[0] == 'oth' and t[1] == NO - 1)]
            invf = k.sb("invf", [128, 144], F32)
            dma("sp", invf[:], I["invf"], W=[invf])
            posi = {"own": k.sb("posi_o", [128, NT], I32), "oth": k.sb("posi_t", [128, NO], I32)}
            posf = {"own": k.sb("posf_o", [128, NT], F32), "oth": k.sb("posf_t", [128, NO], F32)}
            dma("sp", posi["own"][:], I["pos_own"], W=[posi["own"]])
            dma("sp", posi["oth"][:], I["pos_oth"], W=[posi["oth"]])
            for n in ("own", "oth"):
                op("dve", lambda e, n=n: e.tensor_copy(out=posf[n][:], in_=posi[n][:]), R=[posi[n]], W=[posf[n]])
            xt = [k.sb("xt%d" % i, [128, D], F32) for i in range(3)]
            xh = [k.sb("xh%d" % i, [128, D], F32) for i in range(1)]
            hT = [k.sb("hT%d" % i, [128, KC, 128], BF16) for i in range(2)]
            st = k.sb("st", [128, 4, 6], F32)
            mv = k.sb("mv", [128, 2], F32)
            rstd = k.sb("rstd", [128, 1], F32)
            nmr = k.sb("nmr", [128, 1], F32)
            tu = k.sb("tu", [128, 144], F32)
            tk = k.sb("tk", [128, 144], I32)
            tf = k.sb("tf", [128, 144], F32)
            tg = k.sb("tg", [128, 144], F32)
            tabs = [k.sb("tabs%d" % i, [128, 2, 144], F32) for i in range(2)]
            pT = [k.ps("pT%d" % i, [128, 512], F32) for i in range(2)]
            NPZ = 2 if pass_id == 1 else 4
            pz = [k.ps("pz%d" % i, [128, 512], F32) for i in range(NPZ)]
            zo = [k.sb("zo%d" % i, [128, NCOL], BF16) for i in range(2)]
            rt = [k.sb("rt%d" % i, [128, 256], F32) for i in range(4)]
            if pass_id == 1:
                pS = [k.ps("pS%d" % i, [128, 512], F32) for i in range(4)]
                zer = k.sb("zer", [128, 128], BF16)
                op("dve", lambda e: e.memset(zer[:], 0.0), W=[zer])
                for i in range(4):
                    op("pe", lambda e, i=i: e.matmul(pS[i][:], lhsT=zer[:], rhs=W[:, 0, 0:512], start=True, stop=False, skip_group_check=True),
                       R=[zer, W], W=[pS[i]])
                wo = k.sb("wo", [128, NO, 4], F32)
                lgo = k.sb("lgo", [128, 4], F32)
                dist = k.sb("dist", [128, NO], F32)
                bc_load("sp", lgo, I["lg_oth"])
                dma("sp", dist[:], I["dist_oth"], W=[dist])
                for h in range(4):
                    op("dve", lambda e, h=h: e.tensor_scalar(out=wo[:, :, h], in0=dist[:], scalar1=lgo[:, h:h + 1],
                                                             scalar2=None, op0=OP.mult), R=[dist, lgo], W=[wo])
                op("act", lambda e: e.activation(out=wo[:], in_=wo[:], func=AF.Exp), R=[wo], W=[wo])
                kw = [k.sb("kw%d" % i, [128, 1024], BF16) for i in range(2)]
            zc = {"n": 0}

            def stage(ti, part, hook=None):
                kind, idx = tiles[ti]
                src = I["x_own"] if kind == "own" else I["x_oth"]
                X = xt[ti % 3]
                XH = xh[0]
                HT = hT[ti % 2]
                TB = tabs[ti % 2]
                ZO = zo[ti % 2]
                if part == 0:
                    dma("sp", X[:], src[idx * 128:(idx + 1) * 128, :], W=[X])
                elif part == 1:
                    for c4 in range(4):
                        op("dve", lambda e, c4=c4: e.bn_stats(out=st[:, c4, :], in_=X[:, c4 * 512:(c4 + 1) * 512]), R=[X], W=[st])
                    op("dve", lambda e: e.bn_aggr(out=mv[:], in_=st[:].rearrange("p a b -> p (a b)")), R=[st], W=[mv])
                    op("act", lambda e: e.activation(out=rstd[:], in_=mv[:, 1:2], func=AF.Sqrt, bias=LN_EPS_AP[:], scale=1.0),
                       R=[mv, LN_EPS_B], W=[rstd])
                    op("dve", lambda e: e.reciprocal(out=rstd[:], in_=rstd[:]), R=[rstd], W=[rstd])
                    op("dve", lambda e: e.tensor_scalar(out=nmr[:], in0=mv[:, 0:1], scalar1=rstd[:], scalar2=-1.0,
                                                        op0=OP.mult, op1=OP.mult), R=[mv, rstd], W=[nmr])
                    op("act", lambda e: e.activation(out=XH[:], in_=X[:], func=AF.Identity, bias=nmr[:], scale=rstd[:]),
                       R=[X, nmr, rstd], W=[XH])
                    if 'rot' not in KSKIP:
                        P = posf[kind]
                        op("dve", lambda e: e.tensor_scalar(out=tu[:], in0=invf[:], scalar1=P[:, idx:idx + 1], scalar2=None,
                                                            op0=OP.mult), R=[invf, P], W=[tu])
                        op("dve", lambda e: e.tensor_copy(out=tk[:], in_=tu[:]), R=[tu], W=[tk])
                        op("dve", lambda e: e.tensor_tensor(out=tf[:], in0=tu[:], in1=tk[:], op=OP.subtract), R=[tu, tk], W=[tf])
                        op("dve", lambda e: e.scalar_tensor_tensor(out=tg[:], in0=tf[:], scalar=0.5, in1=tf[:], op0=OP.is_gt,
                                                                   op1=OP.subtract), R=[tf], W=[tg])
                        op("act", lambda e: e.activation(out=TB[:, 0, :], in_=tg[:], func=AF.Sin, scale=-2.0 * math.pi),
                           R=[tg], W=[TB])
                        op("dve", lambda e: e.tensor_scalar(out=tf[:], in0=tf[:], scalar1=0.25, scalar2=None, op0=OP.add),
                           R=[tf], W=[tf])
                        op("dve", lambda e: e.scalar_tensor_tensor(out=tg[:], in0=tf[:], scalar=0.5, in1=tf[:], op0=OP.is_gt,
                                                                   op1=OP.subtract), R=[tf], W=[tg])
                        op("act", lambda e: e.activation(out=TB[:, 1, :], in_=tg[:], func=AF.Sin, scale=-2.0 * math.pi),
                           R=[tg], W=[TB])
                        if pass_id == 1:
                            op("dve", lambda e: e.tensor_scalar(out=TB[:, :, 0:128], in0=TB[:, :, 0:128], scalar1=1.0 / 16.0,
                                                                scalar2=None, op0=OP.mult), R=[TB], W=[TB])
                        else:
                            op("dve", lambda e: e.tensor_scalar(out=TB[:, :, 128:144], in0=TB[:, :, 128:144],
                                                                scalar1=128.0 ** -0.5, scalar2=None, op0=OP.mult), R=[TB], W=[TB])
                            XN = X
                            op("pool", lambda e: e.tensor_tensor(out=XN[:], in0=XH[:], in1=g0[:], op=OP.mult), R=[XH, g0], W=[XN])
                            op("pool", lambda e: e.tensor_tensor(out=XN[:], in0=XN[:], in1=b0[:], op=OP.add), R=[XN, b0], W=[XN])
                            dma("pool", S["xn"][idx * 128:(idx + 1) * 128, :], XN[:], R=[XN])
                elif part == 3:
                    if True:
                        for g in range(4):
                            p = pT[g % 2]
                            for j in range(4):
                                c = g * 4 + j
                                op("pe", lambda e, c=c, j=j, p=p: e.transpose(out=p[:, j * 128:(j + 1) * 128],
                                                                              in_=XH[:, c * 128:(c + 1) * 128],
                                                                              identity=ident_f[:]), R=[XH, ident_f], W=[p])
                            for j in range(4):
                                c = g * 4 + j
                                if g % 2 == 0:
                                    op("act", lambda e, c=c, j=j, p=p: e.activation(out=HT[:, c, :], in_=p[:, j * 128:(j + 1) * 128],
                                                                                    func=AF.Identity, bias=cols[:, 1, c:c + 1],
                                                                                    scale=cols[:, 0, c:c + 1]),
                                       R=[p, cols], W=[HT])
                                else:
                                    op("dve", lambda e, c=c, j=j, p=p: e.tensor_scalar(out=HT[:, c, :], in0=p[:, j * 128:(j + 1) * 128],
                                                                                       scalar1=cols[:, 0, c:c + 1],
                                                                                       scalar2=cols[:, 1, c:c + 1],
                                                                                       op0=OP.mult, op1=OP.add),
                                       R=[p, cols], W=[HT])
                else:
                    if 'mm' not in KSKIP:
                        if pass_id == 1:
                            halo = (kind == "oth" and idx < NHALO)
                            slices = list(range(8)) if (kind == "own" or halo) else [4, 5, 6, 7]
                        else:
                            slices = list(range(6))
                        for s in slices:
                            if hook is not None and s == slices[-2]:
                                hook()
                            p = pz[zc["n"] % NPZ]
                            zc["n"] += 1
                            for kc in range(KC):
                                op("pe", lambda e, kc=kc, p=p, s=s: e.matmul(p[:], lhsT=HT[:, kc, :], rhs=W[:, kc, s * 512:(s + 1) * 512],
                                                                             start=(kc == 0), stop=(kc == KC - 1)),
                                   R=[HT, W], W=[p])
                            zs = ZO[:, s * 512:(s + 1) * 512]
                            if pass_id == 1:
                                typ = ("arot", "arot", "copy", "copy", "rrot", "rrot", "copy", "copy")[s]
                            else:
                                typ = ("aqrot", "aqrot", "rrot", "rrot", "silu", "silu")[s]
                            if 'post' in KSKIP:
                                continue
                            if 'rotp' in KSKIP and typ not in ("copy", "silu"):
                                typ = "copy"
                            if 'rota' in KSKIP and typ in ("arot", "aqrot"):
                                typ = "copy"
                            if 'rotr' in KSKIP and typ == "rrot":
                                typ = "copy"
                            if 'pool' in KSKIP and typ not in ("copy", "silu"):
                                typ = typ + "_nopool"
                            if typ == "copy":
                                op("act", lambda e, p=p, zs=zs: e.copy(out=zs, in_=p[:]), R=[p], W=[ZO])
                            elif typ == "silu":
                                op("act", lambda e, p=p, zs=zs: e.activation(out=zs, in_=p[:], func=AF.Silu), R=[p], W=[ZO])
                            elif typ.startswith("a"):
                                sc = 1.0 if typ.startswith("arot") else 128.0 ** -0.5
                                op("act", lambda e, p=p, zs=zs, sc=sc: e.activation(out=zs, in_=p[:], func=AF.Identity, scale=sc),
                                   R=[p], W=[ZO])
                                pv = p[:].rearrange("p (h d) -> p h d", d=128)
                                zv = zs.rearrange("p (h d) -> p h d", d=128)
                                Sn = TB[:, 0, 128:144].unsqueeze(1).to_broadcast([128, 4, 16])
                                Cs = TB[:, 1, 128:144].unsqueeze(1).to_broadcast([128, 4, 16])
                                x1 = pv[:, :, 0:16]
                                x2 = pv[:, :, 16:32]
                                a, b2, c2, d2 = [rt[i][:, 0:64].rearrange("p (h d) -> p h d", d=16) for i in range(4)]
                                op("dve", lambda e, x1=x1, Cs=Cs, a=a: e.tensor_tensor(out=a, in0=x1, in1=Cs, op=OP.mult), R=[p, TB], W=[rt[0]])
                                op("dve", lambda e, x2=x2, Sn=Sn, b2=b2: e.tensor_tensor(out=b2, in0=x2, in1=Sn, op=OP.mult), R=[p, TB], W=[rt[1]])
                                op("dve", lambda e, x2=x2, Cs=Cs, c2=c2: e.tensor_tensor(out=c2, in0=x2, in1=Cs, op=OP.mult), R=[p, TB], W=[rt[2]])
                                op("dve", lambda e, x1=x1, Sn=Sn, d2=d2: e.tensor_tensor(out=d2, in0=x1, in1=Sn, op=OP.mult), R=[p, TB], W=[rt[3]])
                                op(("dve" if "pool" in KSKIP else "pool"), lambda e, zv=zv, a=a, b2=b2: e.tensor_tensor(out=zv[:, :, 0:16], in0=a, in1=b2, op=OP.subtract),
                                   R=[rt[0], rt[1]], W=[ZO])
                                op(("dve" if "pool" in KSKIP else "pool"), lambda e, zv=zv, c2=c2, d2=d2: e.tensor_tensor(out=zv[:, :, 16:32], in0=c2, in1=d2, op=OP.add),
                                   R=[rt[2], rt[3]], W=[ZO])
                            else:
                                pv = p[:].rearrange("p (h t d) -> p h t d", t=2, d=128)
                                zv = zs.rearrange("p (h t d) -> p h t d", t=2, d=128)
                                Sn = TB[:, 0, 0:128].unsqueeze(1).to_broadcast([128, 2, 128])
                                Cs = TB[:, 1, 0:128].unsqueeze(1).to_broadcast([128, 2, 128])
                                x1 = pv[:, :, 0, :]
                                x2 = pv[:, :, 1, :]
                                a, b2, c2, d2 = [rt[i][:].rearrange("p (h d) -> p h d", d=128) for i in range(4)]
                                op("dve", lambda e, x1=x1, Cs=Cs, a=a: e.tensor_tensor(out=a, in0=x1, in1=Cs, op=OP.mult), R=[p, TB], W=[rt[0]])
                                op("dve", lambda e, x2=x2, Sn=Sn, b2=b2: e.tensor_tensor(out=b2, in0=x2, in1=Sn, op=OP.mult), R=[p, TB], W=[rt[1]])
                                op("dve", lambda e, x2=x2, Cs=Cs, c2=c2: e.tensor_tensor(out=c2, in0=x2, in1=Cs, op=OP.mult), R=[p, TB], W=[rt[2]])
                                op("dve", lambda e, x1=x1, Sn=Sn, d2=d2: e.tensor_tensor(out=d2, in0=x1, in1=Sn, op=OP.mult), R=[p, TB], W=[rt[3]])
                                op(("dve" if "pool" in KSKIP else "pool"), lambda e, zv=zv, a=a, b2=b2: e.tensor_tensor(out=zv[:, :, 0, :], in0=a, in1=b2, op=OP.subtract),
                                   R=[rt[0], rt[1]], W=[ZO])
                                op(("dve" if "pool" in KSKIP else "pool"), lambda e, zv=zv, c2=c2, d2=d2: e.tensor_tensor(out=zv[:, :, 1, :], in0=c2, in1=d2, op=OP.add),
                                   R=[rt[2], rt[3]], W=[ZO])
                    if 'out' not in KSKIP:
                        r0 = idx * 128
                        if pass_id == 1:
                            if kind == "own":
                                a0 = 1024 + r0
                                dma("pool", S["ak"][a0:a0 + 128, :], ZO[:, 0:1024], R=[ZO])
                                dma("pool", S["av"][a0:a0 + 128, :], ZO[:, 1024:2048], R=[ZO])
                                dma("pool", S["rk"][r0:r0 + 128, :], ZO[:, 2048:3072], R=[ZO])
                                dma("pool", S["rv"][r0:r0 + 128, :], ZO[:, 3072:4096], R=[ZO])
                            else:
                                if idx < NHALO:
                                    for a0 in (r0, 1024 + SL + r0):
                                        dma("pool", S["ak"][a0:a0 + 128, :], ZO[:, 0:1024], R=[ZO])
                                        dma("pool", S["av"][a0:a0 + 128, :], ZO[:, 1024:2048], R=[ZO])
                                KW = kw[ti % 2]
                                for h in range(4):
                                    op("pool", lambda e, h=h, KW=KW: e.tensor_scalar(out=KW[:, h * 256:(h + 1) * 256],
                                                                                     in0=ZO[:, 2048 + h * 256: 2048 + (h + 1) * 256],
                                                                                     scalar1=wo[:, idx, h:h + 1], scalar2=0.0,
                                                                                     op0=OP.mult, op1=OP.add), R=[ZO, wo], W=[KW])
                                for h in range(4):
                                    for dc in range(2):
                                        op("pe", lambda e, h=h, dc=dc, KW=KW: e.matmul(
                                            pS[h][:, dc * 256:(dc + 1) * 256],
                                            lhsT=KW[:, h * 256 + dc * 128: h * 256 + (dc + 1) * 128],
                                            rhs=ZO[:, 3072 + h * 256: 3072 + (h + 1) * 256],
                                            start=False, stop=False, skip_group_check=True), R=[KW, ZO], W=[pS[h]])
                                if idx == NO - 1:
                                    for h in range(4):
                                        op("pe", lambda e, h=h: e.matmul(pS[h][:], lhsT=zer[:], rhs=W[:, 0, 0:512], start=False,
                                                                         stop=True, skip_group_check=True), R=[zer, W], W=[pS[h]])
                                    for h in range(4):
                                        for dc in range(2):
                                            rb = rt[(2 * h + dc) % 4]
                                            op("act", lambda e, h=h, dc=dc, rb=rb: e.copy(out=rb[:], in_=pS[h][:, dc * 256:(dc + 1) * 256]),
                                               R=[pS[h]], W=[rb])
                                            dma("pool", S["sin"][h, dc], rb[:], R=[rb])
                        else:
                            dma("pool", S["aq"][r0:r0 + 128, :], ZO[:, 0:1024], R=[ZO])
                            dma("pool", S["rq"][r0:r0 + 128, :], ZO[:, 1024:2048], R=[ZO])
                            dma("pool", S["rg"][r0:r0 + 128, :], ZO[:, 2048:3072], R=[ZO])

            stage(0, 0)
            if len(tiles) > 1:
                stage(1, 0)
            stage(0, 1)
            stage(0, 3)
            for ti_ in range(len(tiles)):
                if ti_ + 2 < len(tiles):
                    stage(ti_ + 2, 0)
                hk = None
                if ti_ + 1 < len(tiles):
                    stage(ti_ + 1, 1)
                    hk = (lambda t=ti_: stage(t + 1, 3))
                stage(ti_, 2, hook=hk)


    LN_EPS_B = k.sb("lneps", [128, 1], F32)
    LN_EPS_AP = LN_EPS_B
    op("dve", lambda e: e.memset(LN_EPS_B[:], LN_EPS), W=[LN_EPS_B])

    phaseA(1)
    phaseA(2)
    if stop_after == "A":
        k.close()
        return nc

    S["ao"] = [dscr("ao%d" % p, [SL, 8, 132], F32) for p in range(3)]
    with k.scope():
        negm = k.sb("negm", [128, 4, 128], BF16)
        dma("sp", negm[:], I["negmask"], W=[negm])
        krow = [k.sb("krow%d" % i, [128, 1024], BF16) for i in range(2)]
        qrow = [k.sb("qrow%d" % i, [128, 1024], BF16) for i in range(2)]
        KT = [k.sb("KT%d" % i, [128, 8, 128], BF16) for i in range(3)]
        QT = [k.sb("QT%d" % i, [128, 8, 128], BF16) for i in range(2)]
        VX = [k.sb("VX%d" % i, [128, 8, 132], BF16) for i in range(3)]
        for v in VX:
            op("dve", lambda e, v=v: e.memset(v[:, :, 128:129], 1.0), W=[v])
        pt = [k.sb("pt%d" % i, [128, 256], BF16) for i in range(2)]
        osb = [k.sb("osb%d" % i, [128, 8, 132], F32) for i in range(2)]
        for o_ in osb:
            op("dve", lambda e, o_=o_: e.memset(o_[:], 0.0), W=[o_])
        ptr = [k.ps("ptr%d" % i, [128, 1024], BF16) for i in range(2)]
        pss = [k.ps("pss%d" % i, [128, 512], F32) for i in range(2)]
        pso = [k.ps("pso%d" % i, [128, 512], F32) for i in range(2)]
        cnt = {"tr": 0, "s": 0, "kl": 0, "ql": 0, "q": 0}

        def load_T(rows_ap, rowbuf, dstT, eng):
            dma("sp", rowbuf[:], rows_ap, W=[rowbuf])
            p = ptr[cnt["tr"] % 2]
            cnt["tr"] += 1
            for h in range(8):
                op("pe", lambda e, h=h, p=p: e.transpose(out=p[:, h * 128:(h + 1) * 128], in_=rowbuf[:, h * 128:(h + 1) * 128],
                                                         identity=ident_b[:]), R=[rowbuf, ident_b], W=[p])
            if eng == "act":
                op("act", lambda e, p=p: e.copy(out=dstT[:].rearrange("p h d -> p (h d)"), in_=p[:]), R=[p], W=[dstT])
            else:
                op("dve", lambda e, p=p: e.tensor_copy(out=dstT[:].rearrange("p h d -> p (h d)"), in_=p[:]), R=[p], W=[dstT])

        for pi, dil in enumerate((1, 4, 16)):
            akv = S["ak"].rearrange("(n d) c -> d n c", d=dil)
            avv = S["av"].rearrange("(n d) c -> d n c", d=dil)
            aqv = S["aq"].rearrange("(n d) c -> d n c", d=dil)
            aov = S["ao"][pi].rearrange("(n d) h c -> d n h c", d=dil)
            NQ = SL // dil // 128
            n_base = 1024 // dil - 64
            for r in range(dil):
                def load_k(j):
                    n0 = n_base + 128 * j
                    slot = j % 3
                    load_T(akv[r, n0:n0 + 128, :], krow[cnt["kl"] % 2], KT[slot], "dve")
                    cnt["kl"] += 1
                    dma("sp", VX[slot][:, :, 0:128], avv[r, n0:n0 + 128, :].rearrange("n (h d) -> n h d", d=128), W=[VX[slot]])
                load_k(0)
                for i in range(NQ):
                    load_k(i + 1)
                    Q = QT[i % 2]
                    load_T(aqv[r, 128 * i:128 * (i + 1), :], qrow[cnt["ql"] % 2], Q, "dve")
                    cnt["ql"] += 1
                    O = osb[cnt["q"] % 2]
                    cnt["q"] += 1
                    mA = 2 if i == 0 else 0
                    mB = 3 if i == NQ - 1 else 1
                    def scores(h):
                        ps = pss[cnt["s"] % 2]
                        po = pso[cnt["s"] % 2]
                        P = pt[cnt["s"] % 2]
                        cnt["s"] += 1
                        for half, (slot, mi) in enumerate((((i) % 3, mA), ((i + 1) % 3, mB))):
                            dst = ps[:, half * 128:(half + 1) * 128]
                            op("pe", lambda e, dst=dst, slot=slot, h=h, Q=Q: e.matmul(dst, lhsT=KT[slot][:, h, :], rhs=Q[:, h, :],
                                                                                     start=True, stop=False, skip_group_check=True),
                               R=[KT[slot], Q], W=[ps])
                            op("pe", lambda e, dst=dst, mi=mi: e.matmul(dst, lhsT=ident_b[:], rhs=negm[:, mi, :], start=False,
                                                                        stop=True, skip_group_check=True),
                               R=[ident_b, negm], W=[ps])
                        op("act", lambda e, ps=ps, P=P: e.activation(out=P[:], in_=ps[:, 0:256], func=AF.Exp), R=[ps], W=[P])
                        return (h, po, P)

                    def pvs(hpp):
                        h, po, P = hpp
                        for half, slot in enumerate(((i) % 3, (i + 1) % 3)):
                            op("pe", lambda e, half=half, slot=slot, h=h, po=po, P=P: e.matmul(
                                po[:, 0:129], lhsT=P[:, half * 128:(half + 1) * 128], rhs=VX[slot][:, h, 0:129],
                                start=(half == 0), stop=(half == 1)), R=[P, VX[slot]], W=[po])
                        op("dve", lambda e, h=h, po=po, O=O: e.tensor_copy(out=O[:, h, 0:129], in_=po[:, 0:129]), R=[po], W=[O])

                    prev = None
                    for h in range(8):
                        cur = scores(h)
                        if prev is not None:
                            pvs(prev)
                        prev = cur
                    pvs(prev)
                    dma("pool", aov[r, 128 * i:128 * (i + 1), :, :], O[:], R=[O])
    if stop_after == "B1":
        k.close()
        return nc

    S["sb"] = dscr("sbst", [NT, 128, 4, 512], BF16)
    S["ret"] = dscr("ret", [SL, 1024], BF16)
    with k.scope():
        lgf = k.sb("lgf", [128, 4], F32)
        lgb = k.sb("lgb", [128, 4], F32)
        bc_load("sp", lgf, I["lg_f"])
        bc_load("sp", lgb, I["lg_b"])
        cst = {}
        for n in ("dpos", "dneg", "mfw", "mbw", "idxrow"):
            cst[n] = k.sb(n, [128, 128], F32)
            dma("sp", cst[n][:], I[n], W=[cst[n]])
        idxc = k.sb("idxc", [128, 1], F32)
        dma("sp", idxc[:], I["idxcol"], W=[idxc])
        flg = k.sb("flg", [128, 2], F32)
        dma("sp", flg[:], I["flags"], W=[flg])
        DmT = k.sb("DmT", [128, 4, 128], F32)
        xif = k.sb("xif", [128, 4, 128], BF16)
        xib = k.sb("xib", [128, 4, 128], BF16)
        zf = k.sb("zf", [128, 4], F32)
        zb = k.sb("zb", [128, 4], F32)
        cdf = k.sb("cdf", [128, 4], F32)
        cdb = k.sb("cdb", [128, 4], F32)
        tmpa = k.sb("tmpa", [128, 128], F32)
        tmpb = k.sb("tmpb", [128, 128], F32)
        sc = k.sb("sc", [128, 8], F32)
        zero1 = k.sb("zero1", [128, 1], F32)
        op("dve", lambda e: e.memset(zero1[:], 0.0), W=[zero1])
        for h in range(4):
            op("act", lambda e, h=h: e.activation(out=tmpa[:], in_=cst["dpos"][:], func=AF.Exp, scale=lgf[:, h:h + 1]),
               R=[cst["dpos"], lgf], W=[tmpa])
            op("dve", lambda e: e.tensor_tensor(out=tmpa[:], in0=tmpa[:], in1=cst["mfw"][:], op=OP.mult), R=[tmpa, cst["mfw"]], W=[tmpa])
            op("act", lambda e, h=h: e.activation(out=tmpb[:], in_=cst["dneg"][:], func=AF.Exp, scale=lgb[:, h:h + 1]),
               R=[cst["dneg"], lgb], W=[tmpb])
            op("dve", lambda e: e.tensor_tensor(out=tmpb[:], in0=tmpb[:], in1=cst["mbw"][:], op=OP.mult), R=[tmpb, cst["mbw"]], W=[tmpb])
            op("dve", lambda e, h=h: e.tensor_tensor(out=DmT[:, h, :], in0=tmpa[:], in1=tmpb[:], op=OP.add), R=[tmpa, tmpb], W=[DmT])
            op("act", lambda e, h=h: e.activation(out=xif[:, h, :], in_=cst["idxrow"][:], func=AF.Exp, scale=lgf[:, h:h + 1],
                                                  bias=lgf[:, h:h + 1]), R=[cst["idxrow"], lgf], W=[xif])
            op("dve", lambda e, h=h: e.tensor_scalar(out=sc[:, 0:1], in0=lgb[:, h:h + 1], scalar1=-1.0, scalar2=None, op0=OP.mult),
               R=[lgb], W=[sc])
            op("dve", lambda e, h=h: e.tensor_scalar(out=sc[:, 1:2], in0=lgb[:, h:h + 1], scalar1=128.0, scalar2=None, op0=OP.mult),
               R=[lgb], W=[sc])
            op("act", lambda e, h=h: e.activation(out=xib[:, h, :], in_=cst["idxrow"][:], func=AF.Exp, scale=sc[:, 0:1],
                                                  bias=sc[:, 1:2]), R=[cst["idxrow"], sc], W=[xib])
            op("dve", lambda e, h=h: e.tensor_scalar(out=sc[:, 2:3], in0=lgf[:, h:h + 1], scalar1=-1.0, scalar2=None, op0=OP.mult),
               R=[lgf], W=[sc])
            op("dve", lambda e, h=h: e.tensor_scalar(out=sc[:, 3:4], in0=lgf[:, h:h + 1], scalar1=127.0, scalar2=None, op0=OP.mult),
               R=[lgf], W=[sc])
            op("act", lambda e, h=h: e.activation(out=zf[:, h:h + 1], in_=idxc[:], func=AF.Exp, scale=sc[:, 2:3], bias=sc[:, 3:4]),
               R=[idxc, sc], W=[zf])
            op("act", lambda e, h=h: e.activation(out=zb[:, h:h + 1], in_=idxc[:], func=AF.Exp, scale=lgb[:, h:h + 1], bias=zero1[:]),
               R=[idxc, lgb, zero1], W=[zb])
        op("act", lambda e: e.activation(out=cdf[:], in_=lgf[:], func=AF.Exp, scale=128.0), R=[lgf], W=[cdf])
        op("act", lambda e: e.activation(out=cdb[:], in_=lgb[:], func=AF.Exp, scale=128.0), R=[lgb], W=[cdb])
        Sin = k.sb("Sin", [128, 4, 512], F32)
        dma("sp", Sin[:].rearrange("p h (c n) -> p h c n", c=2), S["sin"].rearrange("h c p n -> p h c n"), W=[Sin])
        Sf = k.sb("Sf", [128, 4, 512], F32)
        Sb = k.sb("Sb", [128, 4, 512], F32)
        op("dve", lambda e: e.tensor_scalar(out=Sf[:], in0=Sin[:], scalar1=flg[:, 0:1], scalar2=None, op0=OP.mult), R=[Sin, flg], W=[Sf])
        op("dve", lambda e: e.tensor_scalar(out=Sb[:], in0=Sin[:], scalar1=flg[:, 1:2], scalar2=None, op0=OP.mult), R=[Sin, flg], W=[Sb])
        Sbf = [k.sb("Sbf%d" % i, [128, 4, 512], BF16) for i in range(2)]
        Sff = [k.sb("Sff%d" % i, [128, 4, 512], BF16) for i in range(2)]
        kt_ = [k.sb("rk%d" % i, [128, 1024], BF16) for i in range(2)]
        vt_ = [k.sb("rv%d" % i, [128, 1024], BF16) for i in range(2)]
        qt_ = [k.sb("rq%d" % i, [128, 1024], BF16) for i in range(2)]
        gt_ = [k.sb("rg%d" % i, [128, 1024], BF16) for i in range(2)]
        kw_ = [k.sb("kwr%d" % i, [128, 1024], BF16) for i in range(2)]
        pS = [k.ps("pS%d" % i, [128, 512], F32) for i in range(2)]
        for ci, c in enumerate(range(NT - 1, -1, -1)):
            SB = Sbf[ci % 2]
            op("act", lambda e, SB=SB: e.copy(out=SB[:], in_=Sb[:]), R=[Sb], W=[SB])
            dma("pool", S["sb"][c], SB[:], R=[SB])
            if c == 0:
                break
            Kt = kt_[ci % 2]
            Vt = vt_[ci % 2]
            KW = kw_[ci % 2]
            dma("sp", Kt[:], S["rk"][c * 128:(c + 1) * 128, :], W=[Kt])
            dma("sp", Vt[:], S["rv"][c * 128:(c + 1) * 128, :], W=[Vt])
            for h in range(4):
                op("pool", lambda e, h=h, KW=KW, Kt=Kt: e.tensor_scalar(out=KW[:, h * 256:(h + 1) * 256], in0=Kt[:, h * 256:(h + 1) * 256],
                                                                        scalar1=zb[:, h:h + 1], scalar2=0.0, op0=OP.mult, op1=OP.add),
                   R=[Kt, zb], W=[KW])
            for h in range(4):
                p = pS[h % 2]
                for dc in range(2):
                    op("pe", lambda e, h=h, dc=dc, p=p, KW=KW, Vt=Vt: e.matmul(
                        p[:, dc * 256:(dc + 1) * 256], lhsT=KW[:, h * 256 + dc * 128:h * 256 + (dc + 1) * 128],
                        rhs=Vt[:, h * 256:(h + 1) * 256], start=True, stop=True, skip_group_check=True), R=[KW, Vt], W=[p])
                op("dve", lambda e, h=h, p=p: e.scalar_tensor_tensor(out=Sb[:, h, :], in0=Sb[:, h, :], scalar=cdb[:, h:h + 1], in1=p[:],
                                                                     op0=OP.mult, op1=OP.add), R=[Sb, cdb, p], W=[Sb])
        k.fence()
        QT2 = [k.sb("QT2%d" % i, [128, 8, 128], BF16) for i in range(2)]
        KT2 = [k.sb("KT2%d" % i, [128, 8, 128], BF16) for i in range(2)]
        Qf = [k.sb("Qf%d" % i, [128, 2, 128], BF16) for i in range(2)]
        Qb = [k.sb("Qb%d" % i, [128, 2, 128], BF16) for i in range(2)]
        PT = [k.sb("PT%d" % i, [128, 128], BF16) for i in range(2)]
        ynm = [k.sb("ynm%d" % i, [128, 256], F32) for i in range(2)]
        ro = [k.sb("ro%d" % i, [128, 1024], BF16) for i in range(2)]
        st2 = k.sb("st2", [128, 6], F32)
        mv2 = k.sb("mv2", [128, 2], F32)
        rs2 = k.sb("rs2", [128, 1], F32)
        nm2 = k.sb("nm2", [128, 1], F32)
        ptq = k.ps("ptq", [128, 1024], BF16)
        ptk = k.ps("ptk", [128, 1024], BF16)
        pa = [k.ps("pa%d" % i, [128, 512], F32) for i in range(2)]
        py = [k.ps("py%d" % i, [128, 512], F32) for i in range(2)]
        SFb = Sff[0]
        op("act", lambda e: e.copy(out=SFb[:], in_=Sf[:]), R=[Sf], W=[SFb])
        hc = 0
        for c in range(NT):
            Kt, Vt, Qt, Gt, KW, SB = kt_[c % 2], vt_[c % 2], qt_[c % 2], gt_[c % 2], kw_[c % 2], Sbf[c % 2]
            RO = ro[c % 2]
            rows = slice(c * 128, (c + 1) * 128)
            dma("sp", Kt[:], S["rk"][rows, :], W=[Kt])
            dma("sp", Vt[:], S["rv"][rows, :], W=[Vt])
            dma("sp", Qt[:], S["rq"][rows, :], W=[Qt])
            dma("sp", Gt[:], S["rg"][rows, :], W=[Gt])
            dma("sp", SB[:], S["sb"][c], W=[SB])
            QT_, KT_ = QT2[c % 2], KT2[c % 2]
            for (src, p, dst, eng) in ((Qt, ptq, QT_, "act"), (Kt, ptk, KT_, "dve")):
                for j in range(8):
                    op("pe", lambda e, j=j, p=p, src=src: e.transpose(out=p[:, j * 128:(j + 1) * 128], in_=src[:, j * 128:(j + 1) * 128],
                                                                      identity=ident_b[:]), R=[src, ident_b], W=[p])
                if eng == "act":
                    op("act", lambda e, p=p, dst=dst: e.copy(out=dst[:].rearrange("p h d -> p (h d)"), in_=p[:]), R=[p], W=[dst])
                else:
                    op("dve", lambda e, p=p, dst=dst: e.tensor_copy(out=dst[:].rearrange("p h d -> p (h d)"), in_=p[:]), R=[p], W=[dst])
            for h in range(4):
                op("pool", lambda e, h=h, KW=KW, Kt=Kt: e.tensor_scalar(out=KW[:, h * 256:(h + 1) * 256], in0=Kt[:, h * 256:(h + 1) * 256],
                                                                        scalar1=zf[:, h:h + 1], scalar2=0.0, op0=OP.mult, op1=OP.add),
                   R=[Kt, zf], W=[KW])
            SFn = Sff[(c + 1) % 2]
            def partA(h, hc):
                A = pa[hc % 2]
                Y = py[hc % 2]
                P_ = PT[hc % 2]
                QF, QB = Qf[hc % 2], Qb[hc % 2]
                YN = ynm[hc % 2]
                for dc in range(2):
                    op("pe", lambda e, h=h, dc=dc, A=A: e.matmul(A[:, 0:128], lhsT=KT_[:, 2 * h + dc, :], rhs=QT_[:, 2 * h + dc, :],
                                                                 start=(dc == 0), stop=(dc == 1)), R=[KT_, QT_], W=[A])
                op("dve", lambda e, h=h, A=A, P_=P_: e.tensor_tensor(out=P_[:], in0=A[:, 0:128], in1=DmT[:, h, :], op=OP.mult),
                   R=[A, DmT], W=[P_])
                op("pool", lambda e, h=h, QF=QF: e.tensor_tensor(out=QF[:], in0=QT_[:, 2 * h:2 * h + 2, :],
                                                                 in1=xif[:, h, :].unsqueeze(1).to_broadcast([128, 2, 128]), op=OP.mult),
                   R=[QT_, xif], W=[QF])
                op("pool", lambda e, h=h, QB=QB: e.tensor_tensor(out=QB[:], in0=QT_[:, 2 * h:2 * h + 2, :],
                                                                 in1=xib[:, h, :].unsqueeze(1).to_broadcast([128, 2, 128]), op=OP.mult),
                   R=[QT_, xib], W=[QB])
                return (h, A, Y, P_, QF, QB, YN)

            def partB(bufs):
                h, A, Y, P_, QF, QB, YN = bufs
                yv = Y[:, 0:256]
                op("pe", lambda e, h=h, yv=yv, P_=P_: e.matmul(yv, lhsT=P_[:], rhs=Vt[:, h * 256:(h + 1) * 256], start=True, stop=False),
                   R=[P_, Vt], W=[Y])
                for dc in range(2):
                    op("pe", lambda e, h=h, dc=dc, yv=yv, QF=QF: e.matmul(yv, lhsT=QF[:, dc, :], rhs=SFb[:, h, dc * 256:(dc + 1) * 256],
                                                                          start=False, stop=False), R=[QF, SFb], W=[Y])
                for dc in range(2):
                    op("pe", lambda e, h=h, dc=dc, yv=yv, QB=QB: e.matmul(yv, lhsT=QB[:, dc, :], rhs=SB[:, h, dc * 256:(dc + 1) * 256],
                                                                          start=False, stop=(dc == 1)), R=[QB, SB], W=[Y])
                p = pS[h % 2]
                for dc in range(2):
                    op("pe", lambda e, h=h, dc=dc, p=p: e.matmul(
                        p[:, dc * 256:(dc + 1) * 256], lhsT=KW[:, h * 256 + dc * 128:h * 256 + (dc + 1) * 128],
                        rhs=Vt[:, h * 256:(h + 1) * 256], start=True, stop=True, skip_group_check=True), R=[KW, Vt], W=[p])
                op("dve", lambda e, h=h, p=p: e.scalar_tensor_tensor(out=Sf[:, h, :], in0=Sf[:, h, :], scalar=cdf[:, h:h + 1], in1=p[:],
                                                                     op0=OP.mult, op1=OP.add), R=[Sf, cdf, p], W=[Sf])
                op("act", lambda e, h=h, SFn=SFn: e.copy(out=SFn[:, h, :], in_=Sf[:, h, :]), R=[Sf], W=[SFn])
                op("dve", lambda e, yv=yv: e.bn_stats(out=st2[:], in_=yv), R=[Y], W=[st2])
                op("dve", lambda e: e.bn_aggr(out=mv2[:], in_=st2[:]), R=[st2], W=[mv2])
                op("act", lambda e: e.activation(out=rs2[:], in_=mv2[:, 1:2], func=AF.Sqrt, bias=LN_EPS_AP[:], scale=1.0),
                   R=[mv2, LN_EPS_B], W=[rs2])
                op("dve", lambda e: e.reciprocal(out=rs2[:], in_=rs2[:]), R=[rs2], W=[rs2])
                op("dve", lambda e: e.tensor_scalar(out=nm2[:], in0=mv2[:, 0:1], scalar1=rs2[:], scalar2=-1.0, op0=OP.mult, op1=OP.mult),
                   R=[mv2, rs2], W=[nm2])
                op("act", lambda e, yv=yv, YN=YN: e.activation(out=YN[:], in_=yv, func=AF.Identity, bias=nm2[:], scale=rs2[:]),
                   R=[Y, nm2, rs2], W=[YN])
                op("pool", lambda e, h=h, YN=YN, RO=RO, Gt=Gt: e.tensor_tensor(out=RO[:, h * 256:(h + 1) * 256], in0=YN[:],
                                                                             in1=Gt[:, h * 256:(h + 1) * 256], op=OP.mult),
                   R=[YN, Gt], W=[RO])

            prevb = None
            for h in range(4):
                curb = partA(h, hc)
                hc += 1
                if prevb is not None:
                    partB(prevb)
                prevb = curb
            partB(prevb)
            SFb = SFn
            dma("pool", S["ret"][rows, :], RO[:], R=[RO])
    if stop_after == "B2":
        k.close()
        return nc

    S["x1"] = dscr("x1", [SL, D], F32)
    S["h2"] = dscr("h2", [SL, D], BF16)
    S["y"] = dscr("ymoe", [NSLOT, D], BF16)
    L_all = k.sb("L_all", [128, NT, 36], F32)
    DEST = k.sb("DEST", [128, NT, 2], I32)
    WT = k.sb("WT", [128, NT, 2], F32)
    if "rt" in dbg:
        S["rt"] = dscr("rt", [SL, 40], F32)
    with k.scope():
        Wo = k.sb("Wo", [128, KC, D], BF16)
        wout_v = I["w_out"].rearrange("(kc p) n -> p kc n", p=128)
        for j in range(4):
            dma("pool", Wo[:, :, j * 512:(j + 1) * 512], wout_v[:, :, j * 512:(j + 1) * 512], W=[Wo])
        with k.scope():
            r1 = k.sb("row_opg1", [128, D], F32)
            bc_load("sp", r1, S["vec"][VEC["opg1"]:VEC["opg1"] + 1, :])
            for kc in range(KC):
                op("pool" if kc % 2 else "dve", lambda e, kc=kc: e.tensor_tensor(out=Wo[:, kc, :], in0=Wo[:, kc, :], in1=r1[:], op=OP.mult),
                   R=[Wo, r1], W=[Wo])
        rows = {}
        for n in ("ln1_g", "ln1_b", "G2", "B2"):
            rows[n] = k.sb("row_" + n, [128, D], F32)
            bc_load("sp", rows[n], S["vec"][VEC[n]:VEC[n] + 1, :])
        Wr = k.sb("Wr", [128, KC, 36], F32)
        dma("sp", Wr[:], I["w_r"].rearrange("(kc p) n -> p kc n", p=128), W=[Wr])
        br = k.sb("br", [128, 36], F32)
        bc_load("sp", br, I["b_r"])
        eoff = k.sb("eoff", [128, 32], F32)
        dma("sp", eoff[:], I["iota_e"], W=[eoff])
        op("dve", lambda e: e.tensor_scalar(out=eoff[:], in0=eoff[:], scalar1=float(CAP), scalar2=None, op0=OP.mult), R=[eoff], W=[eoff])
        tokid = k.sb("tokid", [128, NT], I32)
        dma("sp", tokid[:], I["tokid"], W=[tokid])
        pcolN = k.sb("pcolN", [128, 1], F32)
        dma("sp", pcolN[:], I["idxcol"], W=[pcolN])
        op("dve", lambda e: e.tensor_scalar(out=pcolN[:], in0=pcolN[:], scalar1=float(NSLOT), scalar2=None, op0=OP.add), R=[pcolN], W=[pcolN])
        cntb = k.sb("cntb", [128, 32], F32)
        op("dve", lambda e: e.memset(cntb[:], 0.0), W=[cntb])
        ao_t = [k.sb("ao_t%d" % i, [128, 8, 132], F32) for i in range(6)]
        mix = [k.sb("mix%d" % i, [128, D], BF16) for i in range(2)]
        mixT = [k.sb("mixT%d" % i, [128, KC, 128], BF16) for i in range(2)]
        xr = [k.sb("xr%d" % i, [128, D], F32) for i in range(2)]
        xh1 = [k.sb("xh1_%d" % i, [128, D], F32) for i in range(2)]
        tf32 = k.sb("tf32", [128, D], F32)
        tb16 = k.sb("tb16", [128, D], BF16)
        h2T = k.sb("h2T", [128, KC, 128], F32)
        rden = k.sb("rden", [128, 8], F32)
        st = k.sb("stc", [128, 4, 6], F32)
        mv = k.sb("mvc", [128, 2], F32)
        rstd = k.sb("rstdc", [128, 1], F32)
        nmr = k.sb("nmrc", [128, 1], F32)
        L = k.sb("L", [128, 36], F32)
        r_ = {n: k.sb("r_" + n, shp, F32) for n, shp in (
            ("gmax", [128, 1]), ("ngmax", [128, 1]), ("gone", [128, 4]), ("ge", [128, 4]), ("gsum", [128, 1]), ("pg", [128, 1]),
            ("ss", [128, 8]), ("v0", [128, 1]), ("m0", [128, 8]), ("msk", [128, 8]), ("v1", [128, 1]), ("m1", [128, 8]),
            ("d10", [128, 1]), ("e1", [128, 1]), ("den", [128, 1]), ("M0", [128, 32]), ("M1", [128, 32]), ("M", [128, 32]),
            ("Rk", [128, 32]), ("t32", [128, 32]), ("rank", [128, 2]), ("eo", [128, 2]), ("valid", [128, 2]), ("dg", [128, 2]),
            ("ds", [128, 2]), ("w", [128, 2]))}
        Mb = k.sb("Mb", [128, 32], BF16)
        dsi = k.sb("dsi", [128, 2], I32)
        ptm = [k.ps("ptm%d" % i, [128, 1024], BF16) for i in range(2)]
        po_ = [k.ps("po%d" % i, [128, 512], F32) for i in range(2)]
        pth = [k.ps("pth%d" % i, [128, 512], F32) for i in range(2)]
        prt = k.ps("prt", [128, 512], F32)
        prk = k.ps("prk", [128, 512], F32)
        pcount = 0
        def c_loads_a(i):
            rws_ = slice(i * 128, (i + 1) * 128)
            for p in range(3):
                dma("sp", ao_t[(i % 2) * 3 + p][:], S["ao"][p][rws_, :, :], W=[ao_t[(i % 2) * 3 + p]])
            dma("sp", mix[i % 2][:, 1024:2048], S["ret"][rws_, :], W=[mix[i % 2]])

        def c_loads_x(i):
            dma("sp", xr[i % 2][:], S["xn"][i * 128:(i + 1) * 128, :], W=[xr[i % 2]])

        def c_front(i):
            MIX = mix[i % 2]
            AO = ao_t[(i % 2) * 3:(i % 2) * 3 + 3]
            MT = mixT[i % 2]
            op("dve", lambda e: e.tensor_tensor(out=AO[0][:], in0=AO[0][:], in1=AO[1][:], op=OP.add), R=[AO[0], AO[1]], W=[AO[0]])
            op("dve", lambda e: e.tensor_tensor(out=AO[0][:], in0=AO[0][:], in1=AO[2][:], op=OP.add), R=[AO[0], AO[2]], W=[AO[0]])
            op("dve", lambda e: e.tensor_copy(out=rden[:], in_=AO[0][:, :, 128]), R=[AO[0]], W=[rden])
            op("dve", lambda e: e.reciprocal(out=rden[:], in_=rden[:]), R=[rden], W=[rden])
            op("dve", lambda e, MIX=MIX: e.tensor_tensor(out=MIX[:, 0:1024].rearrange("p (h d) -> p h d", d=128), in0=AO[0][:, :, 0:128],
                                                         in1=rden[:].unsqueeze(2).to_broadcast([128, 8, 128]), op=OP.mult),
               R=[AO[0], rden], W=[MIX])
            for g in range(2):
                p = ptm[g]
                for j in range(8):
                    c = g * 8 + j
                    op("pe", lambda e, c=c, j=j, p=p, MIX=MIX: e.transpose(out=p[:, j * 128:(j + 1) * 128], in_=MIX[:, c * 128:(c + 1) * 128],
                                                                           identity=ident_b[:]), R=[MIX, ident_b], W=[p])
                dstv = MT[:, g * 8:(g + 1) * 8, :].rearrange("p c t -> p (c t)")
                if g == 0:
                    op("act", lambda e, p=p, dstv=dstv: e.copy(out=dstv, in_=p[:]), R=[p], W=[MT])
                else:
                    op("dve", lambda e, p=p, dstv=dstv: e.tensor_copy(out=dstv, in_=p[:]), R=[p], W=[MT])

        def c_mm(i):
            XR = xr[i % 2]
            MT_ = mixT[i % 2]
            for sl in range(4):
                p = po_[(i * 4 + sl) % 2]
                cs_ = slice(sl * 512, (sl + 1) * 512)
                for kc in range(KC):
                    op("pe", lambda e, kc=kc, p=p, cs_=cs_: e.matmul(p[:], lhsT=MT_[:, kc, :], rhs=Wo[:, kc, cs_], start=(kc == 0),
                                                                     stop=(kc == KC - 1)), R=[MT_, Wo], W=[p])
                op("dve", lambda e, p=p, cs_=cs_, XR=XR: e.scalar_tensor_tensor(out=XR[:, cs_], in0=XR[:, cs_], scalar=ALPHA, in1=p[:],
                                                                               op0=OP.mult, op1=OP.add), R=[XR, p], W=[XR])

        def c_ln(i):
            rws = slice(i * 128, (i + 1) * 128)
            XR = xr[i % 2]
            XH = xh1[i % 2]
            for c4 in range(4):
                op("dve", lambda e, c4=c4, XR=XR: e.bn_stats(out=st[:, c4, :], in_=XR[:, c4 * 512:(c4 + 1) * 512]), R=[XR], W=[st])
            op("dve", lambda e: e.bn_aggr(out=mv[:], in_=st[:].rearrange("p a b -> p (a b)")), R=[st], W=[mv])
            op("act", lambda e: e.activation(out=rstd[:], in_=mv[:, 1:2], func=AF.Sqrt, bias=LN_EPS_AP[:], scale=1.0), R=[mv, LN_EPS_B], W=[rstd])
            op("dve", lambda e: e.reciprocal(out=rstd[:], in_=rstd[:]), R=[rstd], W=[rstd])
            op("dve", lambda e: e.tensor_scalar(out=nmr[:], in0=mv[:, 0:1], scalar1=rstd[:], scalar2=-1.0, op0=OP.mult, op1=OP.mult),
               R=[mv, rstd], W=[nmr])
            op("act", lambda e, XR=XR: e.activation(out=XH[:], in_=XR[:], func=AF.Identity, bias=nmr[:], scale=rstd[:]), R=[XR, nmr, rstd], W=[XH])
            op("pool", lambda e: e.tensor_tensor(out=tf32[:], in0=XH[:], in1=rows["ln1_g"][:], op=OP.mult), R=[XH, rows["ln1_g"]], W=[tf32])
            op("pool", lambda e: e.tensor_tensor(out=tf32[:], in0=tf32[:], in1=rows["ln1_b"][:], op=OP.add), R=[tf32, rows["ln1_b"]], W=[tf32])
            dma("pool", S["x1"][rws, :], tf32[:], R=[tf32])
            op("pool", lambda e, XR=XR: e.tensor_tensor(out=XR[:], in0=XH[:], in1=rows["G2"][:], op=OP.mult), R=[XH, rows["G2"]], W=[XR])
            op("pool", lambda e, XR=XR: e.tensor_tensor(out=tb16[:], in0=XR[:], in1=rows["B2"][:], op=OP.add), R=[XR, rows["B2"]], W=[tb16])
            dma("pool", S["h2"][rws, :], tb16[:], R=[tb16])

        def c_router(i):
            XH = xh1[i % 2]
            for g in range(4):
                p = pth[g % 2]
                for j in range(4):
                    c = g * 4 + j
                    op("pe", lambda e, c=c, j=j, p=p: e.transpose(out=p[:, j * 128:(j + 1) * 128], in_=XH[:, c * 128:(c + 1) * 128],
                                                                  identity=ident_f[:]), R=[XH, ident_f], W=[p])
                for j in range(4):
                    c = g * 4 + j
                    if g % 2 == 0:
                        op("act", lambda e, c=c, j=j, p=p: e.activation(out=h2T[:, c, :], in_=p[:, j * 128:(j + 1) * 128], func=AF.Identity,
                                                                        bias=cols[:, 3, c:c + 1], scale=cols[:, 2, c:c + 1]), R=[p, cols], W=[h2T])
                    else:
                        op("dve", lambda e, c=c, j=j, p=p: e.tensor_scalar(out=h2T[:, c, :], in0=p[:, j * 128:(j + 1) * 128],
                                                                           scalar1=cols[:, 2, c:c + 1], scalar2=cols[:, 3, c:c + 1],
                                                                           op0=OP.mult, op1=OP.add), R=[p, cols], W=[h2T])
            for kc in range(KC):
                op("pe", lambda e, kc=kc: e.matmul(prt[:, 0:36], lhsT=h2T[:, kc, :], rhs=Wr[:, kc, :], start=(kc == 0), stop=(kc == KC - 1)),
                   R=[h2T, Wr], W=[prt])
            op("dve", lambda e: e.tensor_tensor(out=L_all[:, i, :], in0=prt[:, 0:36], in1=br[:], op=OP.add), R=[prt, br], W=[L_all])

        c_loads_a(0)
        c_loads_x(0)
        if NT > 1:
            c_loads_a(1)
        c_front(0)
        for i in range(NT):
            if i + 2 < NT:
                c_loads_a(i + 2)
            if i + 1 < NT:
                c_loads_x(i + 1)
            c_mm(i)
            if i + 1 < NT:
                c_front(i + 1)
            c_ln(i)
            if i >= 1:
                c_router(i - 1)
        c_router(NT - 1)

    with k.scope():
        X_ = NT
        trif = k.sb("trif2", [128, 128], F32)
        dma("sp", trif[:], I["tri"], W=[trif])
        trib = k.sb("trib2", [128, 128], BF16)
        op("dve", lambda e: e.tensor_copy(out=trib[:], in_=trif[:]), R=[trif], W=[trib])
        onesb = k.sb("onesb2", [128, 128], BF16)
        op("dve", lambda e: e.memset(onesb[:], 1.0), W=[onesb])
        eoff = k.sb("eoff2", [128, 32], F32)
        dma("sp", eoff[:], I["iota_e"], W=[eoff])
        op("dve", lambda e: e.tensor_scalar(out=eoff[:], in0=eoff[:], scalar1=float(CAP), scalar2=None, op0=OP.mult), R=[eoff], W=[eoff])
        pcolN = k.sb("pcolN2", [128, 1], F32)
        dma("sp", pcolN[:], I["idxcol"], W=[pcolN])
        op("dve", lambda e: e.tensor_scalar(out=pcolN[:], in0=pcolN[:], scalar1=float(NSLOT), scalar2=None, op0=OP.add), R=[pcolN], W=[pcolN])
        tokid = k.sb("tokid2", [128, NT], I32)
        dma("sp", tokid[:], I["tokid"], W=[tokid])
        tokrep = k.sb("tokrep2", [128, NT, 128], I32)
        op("dve", lambda e: e.tensor_copy(out=tokrep[:], in_=tokid[:].unsqueeze(2).to_broadcast([128, NT, 128])), R=[tokid], W=[tokrep])
        B_ = {}

        def T_(n, shp, dt=F32):
            B_[n] = k.sb("q_" + n, shp, dt)
            return B_[n]
        for n, shp in (("gmax", [128, X_]), ("gone", [128, X_, 4]), ("ge", [128, X_, 4]), ("gsum", [128, X_]), ("pg", [128, X_]),
                       ("ss", [128, X_, 8]), ("s2", [128, X_, 8]), ("v0", [128, X_]), ("m0", [128, X_, 8]), ("msk", [128, X_, 8]),
                       ("v1", [128, X_]), ("m1", [128, X_, 8]), ("e1", [128, X_]), ("den", [128, X_]), ("w", [128, X_, 2]),
                       ("M0", [128, X_, 32]), ("M1", [128, X_, 32]), ("M", [128, X_, 32]), ("Rk", [128, X_, 32]), ("t32", [128, X_, 32]),
                       ("base", [128, X_, 32]), ("csum", [128, X_, 32]),
                       ("rank", [128, X_, 2]), ("eo", [128, X_, 2]), ("valid", [128, X_, 2]), ("dg", [128, X_, 2]), ("ds", [128, X_, 2])):
            T_(n, shp)
        Mb = k.sb("q_Mb", [128, X_ * 32], BF16)
        dsi = k.sb("q_dsi", [128, X_, 2], I32)
        pq = [k.ps("pq%d" % i, [128, 512], F32) for i in range(4)]

        def dv(fn, Rb, Wb):
            op("dve", fn, R=[B_[n] if isinstance(n, str) else n for n in Rb], W=[B_[n] if isinstance(n, str) else n for n in Wb])
        G4 = L_all[:, :, 0:4]

        def bc(name, n):
            return B_[name][:].unsqueeze(2).to_broadcast([128, X_, n])
        dv(lambda e: e.tensor_reduce(out=B_["gmax"][:], in_=G4, axis=AX.X, op=OP.max), [L_all], ["gmax"])
        dv(lambda e: e.tensor_tensor(out=B_["gone"][:], in0=G4, in1=bc("gmax", 4), op=OP.is_equal), [L_all, "gmax"], ["gone"])
        dv(lambda e: e.tensor_tensor(out=B_["ge"][:], in0=G4, in1=bc("gmax", 4), op=OP.subtract), [L_all, "gmax"], ["ge"])
        op("act", lambda e: e.activation(out=B_["ge"][:], in_=B_["ge"][:], func=AF.Exp), R=[B_["ge"]], W=[B_["ge"]])
        dv(lambda e: e.tensor_reduce(out=B_["gsum"][:], in_=B_["ge"][:], axis=AX.X, op=OP.add), ["ge"], ["gsum"])
        dv(lambda e: e.reciprocal(out=B_["pg"][:], in_=B_["gsum"][:]), ["gsum"], ["pg"])
        for g in range(4):
            dst = "ss" if g == 0 else "s2"
            dv(lambda e, g=g, dst=dst: e.tensor_tensor(out=B_[dst][:], in0=L_all[:, :, 4 + 8 * g:12 + 8 * g],
                                                     in1=B_["gone"][:, :, g:g + 1].to_broadcast([128, X_, 8]), op=OP.mult),
               [L_all, "gone"], [dst])
            if g > 0:
                dv(lambda e: e.tensor_tensor(out=B_["ss"][:], in0=B_["ss"][:], in1=B_["s2"][:], op=OP.add), ["ss", "s2"], ["ss"])
        dv(lambda e: e.tensor_reduce(out=B_["v0"][:], in_=B_["ss"][:], axis=AX.X, op=OP.max), ["ss"], ["v0"])
        dv(lambda e: e.tensor_tensor(out=B_["m0"][:], in0=B_["ss"][:], in1=bc("v0", 8), op=OP.is_equal), ["ss", "v0"], ["m0"])
        dv(lambda e: e.scalar_tensor_tensor(out=B_["msk"][:].rearrange("p x e -> p (x e)"), in0=B_["m0"][:].rearrange("p x e -> p (x e)"),
                                            scalar=-1e30, in1=B_["ss"][:].rearrange("p x e -> p (x e)"), op0=OP.mult, op1=OP.add),
           ["m0", "ss"], ["msk"])
        dv(lambda e: e.tensor_reduce(out=B_["v1"][:], in_=B_["msk"][:], axis=AX.X, op=OP.max), ["msk"], ["v1"])
        dv(lambda e: e.tensor_tensor(out=B_["m1"][:], in0=B_["msk"][:], in1=bc("v1", 8), op=OP.is_equal), ["msk", "v1"], ["m1"])
        dv(lambda e: e.tensor_tensor(out=B_["e1"][:], in0=B_["v1"][:], in1=B_["v0"][:], op=OP.subtract), ["v1", "v0"], ["e1"])
        op("act", lambda e: e.activation(out=B_["e1"][:], in_=B_["e1"][:], func=AF.Exp), R=[B_["e1"]], W=[B_["e1"]])
        dv(lambda e: e.tensor_scalar(out=B_["den"][:], in0=B_["e1"][:], scalar1=1.0, scalar2=None, op0=OP.add), ["e1"], ["den"])
        dv(lambda e: e.reciprocal(out=B_["den"][:], in_=B_["den"][:]), ["den"], ["den"])
        dv(lambda e: e.tensor_tensor(out=B_["w"][:, :, 0], in0=B_["pg"][:], in1=B_["den"][:], op=OP.mult), ["pg", "den"], ["w"])
        dv(lambda e: e.tensor_tensor(out=B_["w"][:, :, 1], in0=B_["w"][:, :, 0], in1=B_["e1"][:], op=OP.mult), ["w", "e1"], ["w"])
        for (mn, Mn) in (("m0", "M0"), ("m1", "M1")):
            dv(lambda e, mn=mn, Mn=Mn: e.tensor_tensor(out=B_[Mn][:].rearrange("p x (g y) -> p x g y", y=8),
                                                      in0=B_["gone"][:].unsqueeze(3).to_broadcast([128, X_, 4, 8]),
                                                      in1=B_[mn][:].unsqueeze(2).to_broadcast([128, X_, 4, 8]), op=OP.mult),
               ["gone", mn], [Mn])
        dv(lambda e: e.tensor_tensor(out=B_["M"][:], in0=B_["M0"][:], in1=B_["M1"][:], op=OP.add), ["M0", "M1"], ["M"])
        dv(lambda e: e.tensor_copy(out=Mb[:], in_=B_["M"][:].rearrange("p x e -> p (x e)")), ["M"], [Mb])
        NHALF = (X_ * 32 + 511) // 512
        for hh in range(NHALF):
            c0, c1 = hh * 512, min((hh + 1) * 512, X_ * 32)
            op("pe", lambda e, hh=hh, c0=c0, c1=c1: e.matmul(pq[hh][:, 0:c1 - c0], lhsT=trib[:], rhs=Mb[:, c0:c1], start=True, stop=True),
               R=[trib, Mb], W=[pq[hh]])
            op("pe", lambda e, hh=hh, c0=c0, c1=c1: e.matmul(pq[2 + hh][:, 0:c1 - c0], lhsT=onesb[:], rhs=Mb[:, c0:c1], start=True, stop=True),
               R=[onesb, Mb], W=[pq[2 + hh]])
            dv(lambda e, hh=hh, c0=c0, c1=c1: e.tensor_copy(out=B_["Rk"][:].rearrange("p x e -> p (x e)")[:, c0:c1], in_=pq[hh][:, 0:c1 - c0]),
               [pq[hh]], ["Rk"])
            dv(lambda e, hh=hh, c0=c0, c1=c1: e.tensor_copy(out=B_["csum"][:].rearrange("p x e -> p (x e)")[:, c0:c1], in_=pq[2 + hh][:, 0:c1 - c0]),
               [pq[2 + hh]], ["csum"])
        dv(lambda e: e.memset(B_["base"][:, 0, :], 0.0), [], ["base"])
        for i in range(1, X_):
            dv(lambda e, i=i: e.tensor_tensor(out=B_["base"][:, i, :], in0=B_["base"][:, i - 1, :], in1=B_["csum"][:, i - 1, :], op=OP.add),
               ["base", "csum"], ["base"])
        dv(lambda e: e.tensor_tensor(out=B_["Rk"][:], in0=B_["Rk"][:], in1=B_["base"][:], op=OP.add), ["Rk", "base"], ["Rk"])
        for a_, Mn in enumerate(("M0", "M1")):
            dv(lambda e, Mn=Mn: e.tensor_tensor(out=B_["t32"][:], in0=B_[Mn][:], in1=B_["Rk"][:], op=OP.mult), [Mn, "Rk"], ["t32"])
            dv(lambda e, a_=a_: e.tensor_reduce(out=B_["rank"][:, :, a_], in_=B_["t32"][:], axis=AX.X, op=OP.add), ["t32"], ["rank"])
            dv(lambda e, Mn=Mn: e.tensor_tensor(out=B_["t32"][:], in0=B_[Mn][:], in1=eoff[:].unsqueeze(1).to_broadcast([128, X_, 32]),
                                                op=OP.mult), [Mn, eoff], ["t32"])
            dv(lambda e, a_=a_: e.tensor_reduce(out=B_["eo"][:, :, a_], in_=B_["t32"][:], axis=AX.X, op=OP.add), ["t32"], ["eo"])
        dv(lambda e: e.tensor_scalar(out=B_["valid"][:], in0=B_["rank"][:], scalar1=float(CAP), scalar2=None, op0=OP.is_lt), ["rank"], ["valid"])
        dv(lambda e: e.tensor_tensor(out=B_["dg"][:], in0=B_["rank"][:], in1=B_["eo"][:], op=OP.add), ["rank", "eo"], ["dg"])
        dv(lambda e: e.tensor_tensor(out=B_["dg"][:], in0=B_["dg"][:], in1=B_["valid"][:], op=OP.mult), ["dg", "valid"], ["dg"])
        dv(lambda e: e.tensor_tensor(out=WT[:], in0=B_["w"][:], in1=B_["valid"][:], op=OP.mult), ["w", "valid"], [WT])
        dv(lambda e: e.tensor_copy(out=DEST[:], in_=B_["dg"][:]), ["dg"], [DEST])
        dv(lambda e: e.tensor_scalar(out=B_["ds"][:], in0=B_["valid"][:], scalar1=-1.0, scalar2=1.0, op0=OP.mult, op1=OP.add), ["valid"], ["ds"])
        dv(lambda e: e.scalar_tensor_tensor(out=B_["ds"][:].rearrange("p x a -> p (x a)"), in0=B_["ds"][:].rearrange("p x a -> p (x a)"),
                                            scalar=pcolN[:, 0:1], in1=B_["dg"][:].rearrange("p x a -> p (x a)"), op0=OP.mult, op1=OP.add),
           ["ds", "dg", pcolN], ["ds"])
        dv(lambda e: e.tensor_copy(out=dsi[:], in_=B_["ds"][:]), ["ds"], [dsi])
        for i in range(X_):
            for a_ in range(2):
                dma("pool", S["slot"][:, :], tokrep[:, i, :], R=[tokrep, dsi],
                    indirect=dict(out_offset=bass.IndirectOffsetOnAxis(ap=dsi[:, i, a_:a_ + 1], axis=0), in_offset=None))
        if "rt" in dbg:
            dbt = k.sb("dbt", [128, X_, 40], F32)
            dv(lambda e: e.tensor_copy(out=dbt[:, :, 0:36], in_=L_all[:]), [L_all], [dbt])
            dv(lambda e: e.tensor_copy(out=dbt[:, :, 36:38], in_=B_["w"][:]), ["w"], [dbt])
            dv(lambda e: e.tensor_copy(out=dbt[:, :, 38:40], in_=B_["dg"][:]), ["dg"], [dbt])
            dma("sp", S["rt"].rearrange("(x p) c -> p x c", p=128), dbt[:], R=[dbt])
    if stop_after == "C":
        k.close()
        return nc

    CAPB = cfg.CAPB
    with k.scope():
        W1 = [k.sb("W1_%d" % i, [128, KC, FF], BF16) for i in range(2)]
        W3 = [k.sb("W3_%d" % i, [128, KC, FF], BF16) for i in range(2)]
        W2 = [k.sb("W2_%d" % i, [128, FC, D], BF16) for i in range(2)]
        idx = [k.sb("idx%d" % i, [128, CAPB], I32) for i in range(2)]
        xb = [k.sb("xb%d" % i, [128, D], BF16) for i in range(4)]
        xT = k.sb("xT", [128, KC, CAP], BF16)
        sg = [k.sb("sg%d" % i, [128, CAP], F32) for i in range(2)]
        actT = k.sb("actT", [128, FC, CAP], BF16)
        ysb = [k.sb("ysb%d" % i, [128, D], BF16) for i in range(4)]
        ptx = [k.ps("ptx%d" % i, [128, 1024], BF16) for i in range(2)]
        pu1 = k.ps("pu1", [128, 512], F32)
        pu3 = k.ps("pu3", [128, 512], F32)
        pyy = [k.ps("pyy%d" % i, [128, 512], F32) for i in range(4)]

        opg2e = k.sb("opg2e", [128, D], F32)
        bc_load("sp", opg2e, S["vec"][VEC["opg2"]:VEC["opg2"] + 1, :])

        def load_expert(e_):
            b = e_ % 2
            w1v = I["w1"][e_].rearrange("(kc p) f -> p kc f", p=128)
            w3v = I["w3"][e_].rearrange("(kc p) f -> p kc f", p=128)
            w2v = I["w2"][e_].rearrange("(fc p) n -> p fc n", p=128)
            dma("pool", W1[b][:], w1v, W=[W1[b]])
            dma("pool", W3[b][:], w3v, W=[W3[b]])
            for j in range(4):
                dma("pool", W2[b][:, :, j * 512:(j + 1) * 512], w2v[:, :, j * 512:(j + 1) * 512], W=[W2[b]])

        load_expert(0)
        ycount = 0
        for e_ in range(NE):
            b = e_ % 2
            for blk in range(CAPB):
                r0 = e_ * CAP + blk * 128
                dma("sp", idx[b][:, blk:blk + 1], S["slot"][r0:r0 + 128, 0:1], W=[idx[b]], allow_slow_non_contiguous=True)
            for blk in range(CAPB):
                XB = xb[blk % 4]
                dma("pool", XB[:], S["h2"][:, :], R=[idx[b]], W=[XB],
                    indirect=dict(out_offset=None, in_offset=bass.IndirectOffsetOnAxis(ap=idx[b][:, blk:blk + 1], axis=0)))
                for g in range(2):
                    p = ptx[g]
                    for j in range(8):
                        c = g * 8 + j
                        op("pe", lambda e, c=c, j=j, p=p, XB=XB: e.transpose(out=p[:, j * 128:(j + 1) * 128], in_=XB[:, c * 128:(c + 1) * 128],
                                                                            identity=ident_b[:]), R=[XB, ident_b], W=[p])
                    dstv = xT[:, g * 8:(g + 1) * 8, blk * 128:(blk + 1) * 128]
                    srcv = p[:].rearrange("p (c t) -> p c t", t=128)
                    if g == 0:
                        op("act", lambda e, srcv=srcv, dstv=dstv: e.copy(out=dstv, in_=srcv), R=[p], W=[xT])
                    else:
                        op("dve", lambda e, srcv=srcv, dstv=dstv: e.tensor_copy(out=dstv, in_=srcv), R=[p], W=[xT])
            if e_ + 1 < NE:
                load_expert(e_ + 1)
            for m in range(FC):
                for (Wm, pu) in ((W1[b], pu1), (W3[b], pu3)):
                    for kc in range(KC):
                        op("pe", lambda e, kc=kc, m=m, Wm=Wm, pu=pu: e.matmul(pu[:, 0:CAP], lhsT=Wm[:, kc, m * 128:(m + 1) * 128], rhs=xT[:, kc, :],
                                                                              start=(kc == 0), stop=(kc == KC - 1)), R=[Wm, xT], W=[pu])
                SG = sg[m % 2]
                op("act", lambda e, SG=SG: e.activation(out=SG[:], in_=pu1[:, 0:CAP], func=AF.Silu), R=[pu1], W=[SG])
                op("dve", lambda e, m=m, SG=SG: e.tensor_tensor(out=actT[:, m, :], in0=pu3[:, 0:CAP], in1=SG[:], op=OP.mult), R=[pu3, SG], W=[actT])
            for blk in range(CAPB):
                Y = ysb[ycount % 4]
                ycount += 1
                for n in range(4):
                    p = pyy[n]
                    for m in range(FC):
                        op("pe", lambda e, n=n, m=m, p=p, blk=blk: e.matmul(p[:], lhsT=actT[:, m, blk * 128:(blk + 1) * 128],
                                                                           rhs=W2[b][:, m, n * 512:(n + 1) * 512], start=(m == 0),
                                                                           stop=(m == FC - 1)), R=[actT, W2[b]], W=[p])
                    op("dve", lambda e, n=n, p=p, Y=Y: e.tensor_tensor(out=Y[:, n * 512:(n + 1) * 512], in0=p[:],
                                                                       in1=opg2e[:, n * 512:(n + 1) * 512], op=OP.mult), R=[p, opg2e], W=[Y])
                r0 = e_ * CAP + blk * 128
                dma("act", S["y"][r0:r0 + 128, :], Y[:], R=[Y])
    if stop_after == "E":
        k.close()
        return nc

    with k.scope():
        rows = {}
        for n in ("ln2_g", "ln2_b"):
            rows[n] = k.sb("rowf_" + n, [128, D], F32)
            bc_load("sp", rows[n], S["vec"][VEC[n]:VEC[n] + 1, :])
        g0 = [k.sb("g0_%d" % i, [128, D], BF16) for i in range(2)]
        g1 = [k.sb("g1_%d" % i, [128, D], BF16) for i in range(2)]
        gf = [k.sb("gf_%d" % i, [128, D], F32) for i in range(2)]
        x1t = [k.sb("x1t%d" % i, [128, D], F32) for i in range(2)]
        ot = [k.sb("ot%d" % i, [128, D], F32) for i in range(2)]
        st = k.sb("stf", [128, 4, 6], F32)
        mv = k.sb("mvf", [128, 2], F32)
        rstd = k.sb("rstdf", [128, 1], F32)
        nmr = k.sb("nmrf", [128, 1], F32)
        def issue_loads(i):
            for a_, G in enumerate((g0[i % 2], g1[i % 2])):
                dma("pool", G[:], S["y"][:, :], R=[DEST], W=[G],
                    indirect=dict(out_offset=None, in_offset=bass.IndirectOffsetOnAxis(ap=DEST[:, i, a_:a_ + 1], axis=0)))
            dma("sp", x1t[i % 2][:], S["x1"][i * 128:(i + 1) * 128, :], W=[x1t[i % 2]])

        issue_loads(0)
        for i in range(NT):
            rws = slice(i * 128, (i + 1) * 128)
            G0, G1, X1, OT = g0[i % 2], g1[i % 2], x1t[i % 2], ot[i % 2]
            GF = gf[i % 2]
            op("act", lambda e, G0=G0, GF=GF: e.activation(out=GF[:], in_=G0[:], func=AF.Identity, scale=WT[:, i, 0:1]), R=[G0, WT], W=[GF])
            op("dve", lambda e, GF=GF, G1=G1: e.scalar_tensor_tensor(out=GF[:], in0=G1[:], scalar=WT[:, i, 1:2], in1=GF[:], op0=OP.mult,
                                                                    op1=OP.add), R=[GF, G1, WT], W=[GF])
            op("dve", lambda e, GF=GF, X1=X1: e.scalar_tensor_tensor(out=X1[:], in0=X1[:], scalar=ALPHA, in1=GF[:], op0=OP.mult, op1=OP.add),
               R=[X1, GF], W=[X1])
            if i + 1 < NT:
                issue_loads(i + 1)
            for c4 in range(4):
                op("dve", lambda e, c4=c4, X1=X1: e.bn_stats(out=st[:, c4, :], in_=X1[:, c4 * 512:(c4 + 1) * 512]), R=[X1], W=[st])
            op("dve", lambda e: e.bn_aggr(out=mv[:], in_=st[:].rearrange("p a b -> p (a b)")), R=[st], W=[mv])
            op("act", lambda e: e.activation(out=rstd[:], in_=mv[:, 1:2], func=AF.Sqrt, bias=LN_EPS_AP[:], scale=1.0), R=[mv, LN_EPS_B], W=[rstd])
            op("dve", lambda e: e.reciprocal(out=rstd[:], in_=rstd[:]), R=[rstd], W=[rstd])
            op("dve", lambda e: e.tensor_scalar(out=nmr[:], in0=mv[:, 0:1], scalar1=rstd[:], scalar2=-1.0, op0=OP.mult, op1=OP.mult),
               R=[mv, rstd], W=[nmr])
            op("act", lambda e, X1=X1, OT=OT: e.activation(out=OT[:], in_=X1[:], func=AF.Identity, bias=nmr[:], scale=rstd[:]),
               R=[X1, nmr, rstd], W=[OT])
            op("dve", lambda e, OT=OT: e.tensor_tensor(out=OT[:], in0=OT[:], in1=rows["ln2_g"][:], op=OP.mult), R=[OT, rows["ln2_g"]], W=[OT])
            op("pool", lambda e, OT=OT: e.tensor_tensor(out=OT[:], in0=OT[:], in1=rows["ln2_b"][:], op=OP.add), R=[OT, rows["ln2_b"]], W=[OT])
            dma("sp", out_d[rws, :], OT[:], R=[OT])

    k.close()
    return nc


def kernel(**inputs):
    cfg = Cfg()
    maps = prep_inputs(inputs, cfg)
    nc = build(cfg)
    res = run_bass_kernel_spmd(nc, maps, core_ids=list(range(8)))
    out = np.zeros((cfg.B, 2 * cfg.SL, D), np.float32)
    for b in range(cfg.B):
        for hf in range(2):
            out[b, hf * cfg.SL:(hf + 1) * cfg.SL] = res.results[b * 2 + hf]["out"]
    return out
```
